# Optimizing a Trainium2 kernel written in Bass

```python
import jax, jax.numpy as jnp
from jax import lax
import numpy as np


D_MODEL = 2048
BATCH = 8
SEQ = 2048
DEPTH = 1

CHUNK = 64
Q_BLOCK = 128
EPS = 1e-6

H_A = 8
D_LATENT = 128
DH_A = 128
H_IDX = 8
D_IDX = 64
TOPK_MAX = 256

H_R = 8
DK_R = 128
DV_R = 128
ROPE_BASE = 10000.0

D_BRANCH = 1024
N_BRANCH = 2

N_GROUPS = 4
EXP_PER_GROUP = 8
N_EXPERTS = N_GROUPS * EXP_PER_GROUP
TOP_K_EXP = 2
D_EXPERT = 1024
EXPERT_BLOCK = 128

SPLITS = (H_A * D_LATENT,
          D_LATENT,
          H_IDX * D_IDX,
          D_IDX,
          H_IDX,
          H_R * DK_R,
          H_R * DK_R,
          H_R * DV_R,
          H_R * DV_R,
          N_BRANCH * D_MODEL)
D_IN = sum(SPLITS)

kernel_name = "hybrid_dsa_retention_hmoe_block"


def rms_norm(x, g):
    xf = x.astype(jnp.float32)
    y = xf * lax.rsqrt(jnp.mean(xf * xf, axis=-1, keepdims=True) + EPS)
    return (y * g.astype(jnp.float32)).astype(x.dtype)


def head_group_norm(o, g):
    b, s = o.shape[:2]
    of = o.astype(jnp.float32)
    mu = jnp.mean(of, axis=-1, keepdims=True)
    var = jnp.mean(jnp.square(of - mu), axis=-1, keepdims=True)
    y = ((of - mu) * lax.rsqrt(var + EPS)).reshape(b, s, -1)
    return (y * g.astype(jnp.float32)).astype(o.dtype)


def rotary(x, pos):
    half = x.shape[-1] // 2
    freq = ROPE_BASE ** (-jnp.arange(half, dtype=jnp.float32) / half)
    ang = pos.astype(jnp.float32)[:, None] * freq[None, :]
    cos = jnp.cos(ang)[None, :, None, :].astype(x.dtype)
    sin = jnp.sin(ang)[None, :, None, :].astype(x.dtype)
    x1, x2 = x[..., :half], x[..., half:]
    return jnp.concatenate([x1 * cos - x2 * sin, x1 * sin + x2 * cos], axis=-1)


def sparse_indexer_attention(q_lat, kv, q_idx, k_idx, w_idx, w_uv):
    b, s = kv.shape[:2]
    n_sel = min(TOPK_MAX, s // 4)
    nb = s // Q_BLOCK
    key_chunk = jnp.arange(s) // CHUNK
    idx_scale = (H_IDX ** -0.5) * (D_IDX ** -0.5)
    attn_scale = D_LATENT ** -0.5

    def to_blocks(t):
        return jnp.moveaxis(t.reshape((b, nb, Q_BLOCK) + t.shape[2:]), 1, 0)

    def block(args):
        q_blk, qi_blk, wi_blk, start = args
        q_chunk = (start + jnp.arange(Q_BLOCK)) // CHUNK
        dots = jnp.einsum('bqhd,bsd->bqhs', qi_blk, k_idx).astype(jnp.float32)
        score = jnp.einsum('bqh,bqhs->bqs', wi_blk.astype(jnp.float32), jax.nn.relu(dots)) * idx_scale
        admissible = key_chunk[None, :] <= q_chunk[:, None]
        score = jnp.where(admissible[None], score, -jnp.inf)
        _, sel = lax.top_k(score, n_sel)
        kv_sel = jax.vmap(lambda kv_b, sel_b: kv_b[sel_b])(kv, sel)
        valid = key_chunk[sel] <= q_chunk[None, :, None]
        logits = jnp.einsum('bqhc,bqkc->bqhk', q_blk, kv_sel).astype(jnp.float32) * attn_scale
        logits = jnp.where(valid[:, :, None, :], logits, -jnp.inf)
        p = jax.nn.softmax(logits, axis=-1).astype(kv.dtype)
        return jnp.einsum('bqhk,bqkc->bqhc', p, kv_sel)

    starts = jnp.arange(nb) * Q_BLOCK
    o = lax.map(block, (to_blocks(q_lat), to_blocks(q_idx), to_blocks(w_idx), starts))
    o = jnp.moveaxis(o, 0, 1).reshape(b, s, H_A, D_LATENT)
    return jnp.einsum('bshc,hcd->bshd', o, w_uv).reshape(b, s, H_A * DH_A)


def multiscale_retention(q, k, v):
    b, s = q.shape[:2]
    nc = s // CHUNK
    dt = q.dtype
    log_gamma = jnp.log1p(-jnp.exp2(-5.0 - jnp.arange(H_R, dtype=jnp.float32)))
    n = jnp.arange(CHUNK, dtype=jnp.float32)
    diff = n[:, None] - n[None, :]
    decay_in = jnp.where(diff >= 0, jnp.exp(log_gamma[:, None, None] * jnp.maximum(diff, 0.0)), 0.0).astype(dt)
    decay_q = jnp.exp(log_gamma[:, None] * (n + 1.0)).astype(dt)
    decay_k = jnp.exp(log_gamma[:, None] * (CHUNK - 1.0 - n)).astype(dt)
    decay_chunk = jnp.exp(log_gamma * CHUNK).astype(dt)

    def to_chunks(t):
        return t.reshape(b, nc, CHUNK, H_R, t.shape[-1]).transpose(1, 0, 3, 2, 4)

    k = k * (DK_R ** -0.5)

    def step(state, qkv):
        qc, kc, vc = qkv
        inner = jnp.einsum('bhnd,bhmd->bhnm', qc, kc) * decay_in[None]
        o = jnp.einsum('bhnm,bhme->bhne', inner, vc)
        o = o + jnp.einsum('bhnd,bhde->bhne', qc, state) * decay_q[None, :, :, None]
        state = state * decay_chunk[None, :, None, None] + jnp.einsum(
            'bhmd,bhme->bhde', kc * decay_k[None, :, :, None], vc)
        return state, o

    state0 = jnp.zeros((b, H_R, DK_R, DV_R), dt)
    _, o = lax.scan(step, state0, (to_chunks(q), to_chunks(k), to_chunks(v)))
    return o.transpose(1, 0, 3, 2, 4).reshape(b, s, H_R, DV_R)


def hierarchical_moe(x, w_rg, b_rg, w_re, b_re, w_gate, w_up, w_down):
    b, s, d = x.shape
    t = x.reshape(-1, d)
    n_tok = t.shape[0]
    g_prob = jax.nn.softmax((t @ w_rg + b_rg).astype(jnp.float32), axis=-1)
    p_grp, grp = lax.top_k(g_prob, 1)
    e_logits = (t @ w_re + b_re).astype(jnp.float32).reshape(n_tok, N_GROUPS, EXP_PER_GROUP)
    e_logits = jnp.take_along_axis(e_logits, grp[:, :, None], axis=1)[:, 0]
    p_exp, e_loc = lax.top_k(jax.nn.softmax(e_logits, axis=-1), TOP_K_EXP)
    p_exp = p_exp / jnp.sum(p_exp, axis=-1, keepdims=True)
    gate = (p_grp * p_exp).astype(x.dtype)
    expert = (grp * EXP_PER_GROUP + e_loc).astype(jnp.int32)

    n_asg = n_tok * TOP_K_EXP
    e_flat = expert.reshape(-1)
    tok_flat = jnp.repeat(jnp.arange(n_tok, dtype=jnp.int32), TOP_K_EXP)
    gate_flat = gate.reshape(-1)
    order = jnp.argsort(e_flat)
    e_sorted = e_flat[order]
    counts = jnp.bincount(e_flat, length=N_EXPERTS)
    padded = (counts + EXPERT_BLOCK - 1) // EXPERT_BLOCK * EXPERT_BLOCK
    pad_end = jnp.cumsum(padded)
    pad_start = pad_end - padded
    cnt_start = jnp.cumsum(counts) - counts
    rank = jnp.arange(n_asg) - cnt_start[e_sorted]
    dest = (pad_start[e_sorted] + rank).astype(jnp.int32)
    n_rows = -(-(n_asg + N_EXPERTS * (EXPERT_BLOCK - 1)) // EXPERT_BLOCK) * EXPERT_BLOCK
    n_blocks = n_rows // EXPERT_BLOCK
    row_tok = jnp.full((n_rows,), n_tok, jnp.int32).at[dest].set(tok_flat[order])
    row_gate = jnp.zeros((n_rows,), x.dtype).at[dest].set(gate_flat[order])
    block_expert = jnp.minimum(
        jnp.searchsorted(pad_end, jnp.arange(n_blocks) * EXPERT_BLOCK, side='right'), N_EXPERTS - 1)
    t_pad = jnp.concatenate([t, jnp.zeros((1, d), t.dtype)], axis=0)

    def block(args):
        rows, e = args
        xb = t_pad[rows]
        hmid = jax.nn.silu(xb @ w_gate[e]) * (xb @ w_up[e])
        return hmid @ w_down[e]

    y = lax.map(block, (row_tok.reshape(n_blocks, EXPERT_BLOCK), block_expert))
    y = y.reshape(n_rows, d) * row_gate[:, None]
    out = jnp.zeros((n_tok + 1, d), x.dtype).at[row_tok].add(y)[:n_tok]
    return out.reshape(b, s, d)


def setup_inputs(seed: int = 0) -> dict:
    key = jax.random.key(seed)
    ks = jax.random.split(key, 20)
    f32 = jnp.float32

    def nrm(k, shape, scale):
        return jax.random.normal(k, shape, f32) * scale

    def gain(k, shape):
        return 1.0 + 0.01 * jax.random.normal(k, shape, f32)

    return {
        'x': jax.random.normal(ks[0], (BATCH, SEQ, D_MODEL), f32),
        'g_mix_norm': gain(ks[1], (DEPTH, D_MODEL)),
        'w_in': nrm(ks[2], (DEPTH, D_MODEL, D_IN), D_MODEL ** -0.5),
        'g_kv': gain(ks[3], (DEPTH, D_LATENT)),
        'w_uv': nrm(ks[4], (DEPTH, H_A, D_LATENT, DH_A), D_LATENT ** -0.5),
        'g_ret': gain(ks[5], (DEPTH, H_R * DV_R)),
        'w_branch': nrm(ks[6], (DEPTH, N_BRANCH, D_BRANCH, D_MODEL), D_BRANCH ** -0.5),
        'w_out': nrm(ks[7], (DEPTH, D_MODEL, D_MODEL), D_MODEL ** -0.5),
        'g_ffn_norm': gain(ks[8], (DEPTH, D_MODEL)),
        'w_router_group': nrm(ks[9], (DEPTH, D_MODEL, N_GROUPS), D_MODEL ** -0.5),
        'b_router_group': nrm(ks[10], (DEPTH, N_GROUPS), 0.01),
        'w_router_expert': nrm(ks[11], (DEPTH, D_MODEL, N_EXPERTS), D_MODEL ** -0.5),
        'b_router_expert': nrm(ks[12], (DEPTH, N_EXPERTS), 0.01),
        'w_expert_gate': nrm(ks[13], (DEPTH, N_EXPERTS, D_MODEL, D_EXPERT), D_MODEL ** -0.5),
        'w_expert_up': nrm(ks[14], (DEPTH, N_EXPERTS, D_MODEL, D_EXPERT), D_MODEL ** -0.5),
        'w_expert_down': nrm(ks[15], (DEPTH, N_EXPERTS, D_EXPERT, D_MODEL), D_EXPERT ** -0.5),
        'g_final': gain(ks[16], (D_MODEL,)),
    }


def reference(x, g_mix_norm, w_in, g_kv, w_uv, g_ret, w_branch, w_out, g_ffn_norm,
              w_router_group, b_router_group, w_router_expert, b_router_expert,
              w_expert_gate, w_expert_up, w_expert_down, g_final):
    b, s, d = x.shape
    pos = jnp.arange(s)
    split_points = np.cumsum(np.array(SPLITS))[:-1].tolist()
    h = x
    for l in range(DEPTH):
        xn = rms_norm(h, g_mix_norm[l])
        proj = xn @ w_in[l]
        (q_lat, c_kv, q_idx, k_idx, w_idx, q_r, k_r, v_r, gate_r, gate_br) = jnp.split(
            proj, split_points, axis=-1)

        kv = rms_norm(c_kv, g_kv[l])
        o_a = sparse_indexer_attention(
            q_lat.reshape(b, s, H_A, D_LATENT), kv,
            q_idx.reshape(b, s, H_IDX, D_IDX), k_idx, w_idx, w_uv[l])

        q_r = rotary(q_r.reshape(b, s, H_R, DK_R), pos)
        k_r = rotary(k_r.reshape(b, s, H_R, DK_R), pos)
        ret = multiscale_retention(q_r, k_r, v_r.reshape(b, s, H_R, DV_R))
        o_b = jax.nn.silu(gate_r) * head_group_norm(ret, g_ret[l])

        branches = jnp.einsum('bsnc,ncd->bsnd', jnp.stack([o_a, o_b], axis=2), w_branch[l])
        gates = jax.nn.sigmoid(gate_br.reshape(b, s, N_BRANCH, d))
        mixed = jnp.einsum('bsnd,bsnd->bsd', gates, branches)
        h = h + mixed @ w_out[l]

        h = h + hierarchical_moe(rms_norm(h, g_ffn_norm[l]),
                                 w_router_group[l], b_router_group[l],
                                 w_router_expert[l], b_router_expert[l],
                                 w_expert_gate[l], w_expert_up[l], w_expert_down[l])
    return rms_norm(h, g_final)
```

```python
import os
from contextlib import ExitStack
import numpy as np
import ml_dtypes
import concourse.bass as bass
import concourse.mybir as mybir
from concourse.bass_utils import run_bass_kernel_spmd

F32 = mybir.dt.float32
BF16 = mybir.dt.bfloat16
I32 = mybir.dt.int32
U32 = mybir.dt.uint32
ALU = mybir.AluOpType
AF = mybir.ActivationFunctionType
AX = mybir.AxisListType

S = 2048
D = 2048
NT = 16
EPS = 1e-6
D_IN = 9928
C_QLAT, C_CKV, C_QIDX, C_KIDX, C_WIDX, C_QR, C_KR, C_VR, C_GR, C_GBR = (
    0, 1024, 1152, 1664, 1728, 1736, 2760, 3784, 4808, 5832)
NEXP = 32
CAP = 256
FEXP = 1024
NEG = -1.0e30
MBIG = 30000.0


class _Op:
    __slots__ = ("eng", "fn", "dma", "deps", "signal", "token", "idx")


class Sched:
    ENGS = ("pe", "dve", "act", "pool", "sp")

    def __init__(self, nc, stack, ndma=8):
        self.nc = nc
        self.ops = []
        self.lastw = {}
        self.readers = {}
        self.ndma = ndma
        self.csem = {e: stack.enter_context(nc.semaphore("cs_" + e)) for e in ("pe", "dve", "act", "pool")}
        self.dsem = {e: [stack.enter_context(nc.semaphore("ds_%s%d" % (e, i))) for i in range(ndma)]
                     for e in ("sp", "act", "pool")}
        self.ccount = {e: 0 for e in self.csem}
        self.qcount = {e: 0 for e in self.dsem}
        self.suses = {e: [0] * ndma for e in self.dsem}
        self.sprev = {e: [None] * ndma for e in self.dsem}
        self.waited = {e: {} for e in self.ENGS}
        self.flushed = 0

    def add(self, eng, fn, reads=(), writes=(), dma=False):
        op = _Op()
        op.eng, op.fn, op.dma = eng, fn, dma
        op.deps = set()
        op.signal = dma
        op.token = None
        op.idx = len(self.ops)

        def dep(p, raw):
            if p is op:
                return
            if (not p.dma) and p.eng == eng and (not dma):
                if eng == "pe":
                    return
            op.deps.add(p)

        for k in reads:
            p = self.lastw.get(k)
            if p is not None:
                dep(p, True)
        for k in writes:
            p = self.lastw.get(k)
            if p is not None:
                dep(p, False)
            for r in self.readers.get(k, ()):
                dep(r, False)
        for k in reads:
            self.readers.setdefault(k, []).append(op)
        for k in writes:
            self.lastw[k] = op
            self.readers[k] = []
        self.ops.append(op)
        return op

    def flush(self):
        nc = self.nc
        ops = self.ops[self.flushed:]
        for op in ops:
            if op.dma:
                e = op.eng
                slot = self.qcount[e] % self.ndma
                self.qcount[e] += 1
                prev = self.sprev[e][slot]
                if prev is not None:
                    op.deps.add(prev)
                self.suses[e][slot] += 1
                op.token = (self.dsem[e][slot], 16 * self.suses[e][slot])
                self.sprev[e][slot] = op
        for op in ops:
            for d in op.deps:
                d.signal = True
        for op in ops:
            if op.signal and not op.dma and op.token is None:
                self.ccount[op.eng] += 1
                op.token = (self.csem[op.eng], self.ccount[op.eng])
        fence = _Op()
        fence.eng, fence.fn, fence.dma, fence.signal, fence.token = "sp", None, False, False, None
        fence.deps = set(op for op in ops if op.dma)
        fence.idx = -1
        by_eng = {e: [] for e in self.ENGS}
        for op in ops:
            by_eng[op.eng].append(op)
        by_eng["sp"].append(fence)
        self.flushed = len(self.ops)

        def emit(ename, e):
            waited = self.waited[ename]
            for op in by_eng[ename]:
                need = {}
                for d in op.deps:
                    if d.token is None:
                        continue
                    sem, val = d.token
                    key = id(sem)
                    if waited.get(key, 0) < val and need.get(key, (None, 0))[1] < val:
                        need[key] = (sem, val)
                for key, (sem, val) in need.items():
                    e.wait_ge(sem, val)
                    waited[key] = val
                if op.fn is None:
                    continue
                ins = op.fn(e)
                if op.signal:
                    sem, _ = op.token
                    ins.then_inc(sem, 16 if op.dma else 1)

        with nc.Block() as block:
            if by_eng["pe"]:
                @block.tensor
                def _(e):
                    emit("pe", e)
            if by_eng["dve"]:
                @block.vector
                def _(e):
                    emit("dve", e)
            if by_eng["act"]:
                @block.scalar
                def _(e):
                    emit("act", e)
            if by_eng["pool"]:
                @block.gpsimd
                def _(e):
                    emit("pool", e)

            @block.sync
            def _(e):
                emit("sp", e)


CB = {}
CF = {}


def _layout():
    off = 0
    for name, w in (("ident", 128), ("ident8", 1024), ("ones", 128), ("utri", 128)):
        CB[name] = (off, w)
        off += w
    CB["_n"] = off
    off = 0
    for name, w in (("cos", NT * 64), ("sin", NT * 64), ("dq", 8 * 128), ("gk", 8), ("kdec", 8),
                    ("cmask", 128), ("ebase", NEXP), ("iota_e", NEXP)):
        CF[name] = (off, w)
        off += w
    CF["_n"] = off


_layout()


def make_consts():
    cb = np.zeros((128, CB["_n"]), np.float32)
    eye = np.eye(128, dtype=np.float32)
    o, w = CB["ident"]; cb[:, o:o + w] = eye
    o, w = CB["ident8"]; cb[:, o:o + w] = np.tile(eye, (1, 8))
    o, w = CB["ones"]; cb[:, o:o + w] = 1.0
    o, w = CB["utri"]; cb[:, o:o + w] = np.triu(np.ones((128, 128), np.float32), 1)
    cf = np.zeros((128, CF["_n"]), np.float32)
    half = 64
    freq = (10000.0 ** (-np.arange(half, dtype=np.float32) / half)).astype(np.float32)
    pos = np.arange(S, dtype=np.float32)
    ang = (pos[:, None] * freq[None, :]).astype(np.float32)
    cos = np.cos(ang).astype(np.float32).reshape(NT, 128, 64).transpose(1, 0, 2).reshape(128, NT * 64)
    sin = np.sin(ang).astype(np.float32).reshape(NT, 128, 64).transpose(1, 0, 2).reshape(128, NT * 64)
    o, w = CF["cos"]; cf[:, o:o + w] = cos
    o, w = CF["sin"]; cf[:, o:o + w] = sin
    lg = np.log1p(-np.exp2(-5.0 - np.arange(8, dtype=np.float64)))
    n = np.arange(128, dtype=np.float64)
    dq = np.exp(lg[:, None] * (n[None, :] + 1.0)) * (128.0 ** -0.5)
    o, w = CF["dq"]; cf[:, o:o + w] = dq.reshape(1, 1024)
    o, w = CF["gk"]; cf[:, o:o + w] = np.exp(-lg[None, :] * (n[:, None] + 1.0))
    o, w = CF["kdec"]; cf[:, o:o + w] = np.exp(lg[None, :] * (127.0 - n[:, None]))
    o, w = CF["cmask"]; cf[:, o:o + w] = np.triu(np.ones((128, 128)), 0)
    o, w = CF["ebase"]; cf[:, o:o + w] = (np.arange(NEXP) * CAP)[None, :]
    o, w = CF["iota_e"]; cf[:, o:o + w] = np.arange(NEXP)[None, :]
    g128 = [float(np.exp(lg[h] * 128.0)) for h in range(8)]
    return cb.astype(ml_dtypes.bfloat16), cf.astype(np.float32), g128


_G128 = make_consts()[2]


def build(debug=False, upto=99):
    nc = bass.Bass("TRN2", target_bir_lowering=False)
    dk = "ExternalOutput" if debug else "Internal"

    def din(name, shape, dt=F32):
        return nc.dram_tensor(name, list(shape), dt, kind="ExternalInput").ap()

    def dscr(name, shape, dt):
        return nc.dram_tensor(name, list(shape), dt, kind=dk).ap()

    x = din("x", [S, D])
    g_mix = din("g_mix_norm", [1, D])
    w_in = din("w_in", [D, D_IN])
    g_kv = din("g_kv", [1, 128])
    w_uv = din("w_uv", [8, 128, 128])
    g_ret = din("g_ret", [1, 1024])
    w_branch = din("w_branch", [2, 1024, D])
    w_out = din("w_out", [D, D])
    g_ffn = din("g_ffn_norm", [1, D])
    w_rg = din("w_router_group", [D, 4])
    b_rg = din("b_router_group", [1, 4])
    w_re = din("w_router_expert", [D, NEXP])
    b_re = din("b_router_expert", [1, NEXP])
    w_eg = din("w_expert_gate", [NEXP, D, FEXP])
    w_eu = din("w_expert_up", [NEXP, D, FEXP])
    w_ed = din("w_expert_down", [NEXP, FEXP, D])
    g_fin = din("g_final", [1, D])
    cbd = din("cb", [128, CB["_n"]], BF16)
    cfd = din("cf", [128, CF["_n"]], F32)
    out = nc.dram_tensor("out", [S, D], F32, kind="ExternalOutput").ap()

    qlatT_d = dscr("qlatT_d", [1024, S], BF16)
    qidxT_d = dscr("qidxT_d", [512, S], BF16)
    qrot_d = dscr("qrot_d", [S, 1024], BF16)
    krot_d = dscr("krot_d", [S, 1024], BF16)
    kdec_d = dscr("kdec_d", [S, 1024], BF16)
    vr_d = dscr("vr_d", [S, 1024], BF16)
    gr_d = dscr("gr_d", [S, 1024], BF16)
    gbT_d = dscr("gbT_d", [4096, S], BF16)
    oaT_d = dscr("oaT_d", [1024, S], BF16)
    obT_d = dscr("obT_d", [1024, S], BF16)
    h1_d = dscr("h1_d", [S, D], F32)
    xn2_d = dscr("xn2_d", [S, D], BF16)
    xdisp_d = dscr("xdisp_d", [NEXP * CAP, D], BF16)
    ydisp_d = dscr("ydisp_d", [NEXP * CAP, D], F32)
    dbg_d = dscr("dbg_d", [128, 4096], F32)
    NF32 = int(os.environ.get("MOE_F32_EXPERTS", 16))
    NCV = NEXP - NF32
    wgb_d = nc.dram_tensor("wgb_d", [NCV, D, FEXP], BF16).ap()
    wub_d = nc.dram_tensor("wub_d", [NCV, D, FEXP], BF16).ap()
    wdb_d = nc.dram_tensor("wdb_d", [NCV, FEXP, D], BF16).ap()

    with ExitStack() as P:
        sch = Sched(nc, P)
        add = sch.add

        def sb(st, name, shape, dt):
            return st.enter_context(nc.sbuf_tensor(name, list(shape), dt))

        def ps(st, name, shape, dt):
            return st.enter_context(nc.psum_tensor(name, list(shape), dt))

        def conv_gen():
            for ex in range(NF32, NEXP):
                for kind, src, dst in (("g", w_eg, wgb_d), ("u", w_eu, wub_d), ("d", w_ed, wdb_d)):
                    add("pool", lambda e, ex=ex, src=src, dst=dst: e.dma_start(
                        out=dst[ex - NF32].rearrange("(c p) n -> p c n", p=128), in_=src[ex].rearrange("(c p) n -> p c n", p=128)),
                        writes=[("wconv", ex, kind)], dma=True)
                    yield
        conv_it = conv_gen()

        def conv(n):
            if upto < 7:
                return
            for _ in range(n):
                try:
                    next(conv_it)
                except StopIteration:
                    return

        cb = sb(P, "cb_sb", [128, CB["_n"]], BF16)
        cf = sb(P, "cf_sb", [128, CF["_n"]], F32)
        add("sp", lambda e: e.dma_start(out=cb[:], in_=cbd), writes=["cb"], dma=True)
        add("sp", lambda e: e.dma_start(out=cf[:], in_=cfd), writes=["cf"], dma=True)

        def cbs(name):
            o, w = CB[name]
            return cb[:, o:o + w]

        def cfs(name):
            o, w = CF[name]
            return cf[:, o:o + w]

        ident = cbs("ident")

        def dump(name, src, shape, dt, reads):
            dd = nc.dram_tensor("dbg_" + name, list(shape), dt, kind="ExternalOutput").ap()
            add("sp", lambda e: e.dma_start(out=dd, in_=src), reads=reads, writes=["dbg_" + name], dma=True)

        KC = ExitStack()
        kvT = sb(KC, "kvT", [128, S], BF16)
        kvtok = sb(KC, "kvtok", [128, NT, 128], BF16)
        kidxT = sb(KC, "kidxT", [128, S], BF16)
        wabs = sb(KC, "wabs", [128, NT, 8], F32)
        wsgn = sb(KC, "wsgn", [128, NT, 8], F32)

        with ExitStack() as st:
            xnT = sb(st, "xnT", [128, NT, S], BF16)
            gbc = sb(st, "gbc", [128, D], F32)
            gkvbc = sb(st, "gkvbc", [128, 128], F32)
            ss = sb(st, "ss", [128, NT], F32)
            rstd = sb(st, "rstd", [128, NT], F32)
            xt = [sb(st, "xt%d" % i, [128, D], F32) for i in range(2)]
            xnb = [sb(st, "xnb%d" % i, [128, D], BF16) for i in range(2)]
            junk = sb(st, "junk", [128, D], BF16)
            pT = [ps(st, "pT%d" % i, [128, 1024], BF16) for i in range(2)]
            pG = [ps(st, "pG%d" % i, [128, 512], F32) for i in range(4)]
            pM = [ps(st, "pM%d" % i, [128, 512], F32) for i in range(2)]

            add("sp", lambda e: e.dma_start(out=gbc[:], in_=g_mix[0:1, :].to_broadcast([128, D])), writes=["gbc"], dma=True)
            zt = sb(st, "zt", [128, D], BF16)
            add("pool", lambda e: e.memset(zt[:], 0.0), writes=["zt"])
            for zi in range(4):
                add("sp", lambda e, zi=zi: e.dma_start(
                    out=xdisp_d[zi * 2048:(zi + 1) * 2048, :].rearrange("(r p) d -> p r d", p=128),
                    in_=zt[:].unsqueeze(1).to_broadcast([128, 16, D])),
                    reads=["zt"], writes=["xdisp_d"], dma=True)
            add("sp", lambda e: e.dma_start(out=gkvbc[:], in_=g_kv[0:1, :].to_broadcast([128, 128])), writes=["gkvbc"], dma=True)
            for t in range(NT):
                b = t % 2
                add("sp", lambda e, t=t, b=b: e.dma_start(out=xt[b][:], in_=x[t * 128:(t + 1) * 128, :]),
                    writes=[("xt", b)], dma=True)
                add("act", lambda e, t=t, b=b: e.activation(out=junk[:], in_=xt[b][:], func=AF.Square,
                                                            accum_out=ss[:, t:t + 1]),
                    reads=[("xt", b)], writes=["junk", ("ss", t)])
                add("act", lambda e, t=t: e.activation(out=rstd[:, t:t + 1], in_=ss[:, t:t + 1], func=AF.Sqrt,
                                                       scale=1.0 / D, bias=EPS),
                    reads=[("ss", t)], writes=[("rstd", t)])
                add("dve", lambda e, t=t: e.reciprocal(out=rstd[:, t:t + 1], in_=rstd[:, t:t + 1]),
                    reads=[("rstd", t)], writes=[("rstd", t)])
                add("dve", lambda e, t=t, b=b: e.scalar_tensor_tensor(out=xnb[b][:], in0=xt[b][:], scalar=rstd[:, t:t + 1],
                                                                      in1=gbc[:], op0=ALU.mult, op1=ALU.mult),
                    reads=[("xt", b), ("rstd", t), "gbc"], writes=[("xnb", b)])

                def tr(e, t=t, b=b):
                    ins = None
                    for c in range(16):
                        ins = e.transpose(out=pT[c // 8][:, (c % 8) * 128:(c % 8 + 1) * 128],
                                          in_=xnb[b][:, c * 128:(c + 1) * 128], identity=ident)
                    return ins
                add("pe", tr, reads=[("xnb", b), "cb"], writes=["pT0", "pT1"])
                add("act", lambda e, t=t: e.activation(out=xnT[:, 0:8, t * 128:(t + 1) * 128],
                                                       in_=pT[0][:].rearrange("p (c n) -> p c n", c=8), func=AF.Copy),
                    reads=["pT0"], writes=[("xnT", t)])
                add("dve", lambda e, t=t: e.tensor_copy(out=xnT[:, 8:16, t * 128:(t + 1) * 128],
                                                        in_=pT[1][:].rearrange("p (c n) -> p c n", c=8)),
                    reads=["pT1"], writes=[("xnT", t)])
            XN_ALL = [("xnT", t) for t in range(NT)]

            NW = 3
            wr = [sb(st, "wr%d" % i, [128, 16, 512], BF16) for i in range(NW)]
            stg = [sb(st, "stg%d" % i, [128, 2048], BF16) for i in range(2)]
            stg2 = [sb(st, "stgb%d" % i, [128, 2048], BF16) for i in range(2)]
            tmp = [sb(st, "rtmp%d" % i, [128, 4, 64], F32) for i in range(4)]
            wcnt = [0]
            pcnt = [0]
            scnt = [0]
            ecnt = [0]

            def load_w(c0, width, dup=False):
                slot = wcnt[0] % NW
                wcnt[0] += 1
                src = w_in[:, c0:c0 + width].rearrange("(c p) n -> p c n", p=128)
                if dup:
                    add("pool", lambda e: e.dma_start(out=wr[slot][:, :, 0:width], in_=src), writes=[("wr", slot)], dma=True)
                    add("pool", lambda e: e.dma_start(out=wr[slot][:, :, width:2 * width], in_=src), writes=[("wr", slot)], dma=True)
                else:
                    add("pool", lambda e: e.dma_start(out=wr[slot][:, :, 0:width], in_=src), writes=[("wr", slot)], dma=True)
                if wcnt[0] > 3:
                    conv(1)
                return slot

            def gemm_T(slot, t, width):
                pb = pcnt[0] % 4
                pcnt[0] += 1

                def f(e):
                    ins = None
                    for c in range(16):
                        ins = e.matmul(pG[pb][:, 0:width], lhsT=xnT[:, c, t * 128:(t + 1) * 128], rhs=wr[slot][:, c, 0:width],
                                       start=(c == 0), stop=(c == 15))
                    return ins
                add("pe", f, reads=[("wr", slot), ("xnT", t)], writes=[("pG", pb)])
                return pb

            def gemm_F(slot, j, tb):
                pb = pcnt[0] % 4
                pcnt[0] += 1

                def f(e):
                    ins = None
                    for c in range(16):
                        ins = e.matmul(pG[pb][:, :], lhsT=wr[slot][:, c, j * 128:(j + 1) * 128], rhs=xnT[:, c, tb * 512:(tb + 1) * 512],
                                       start=(c == 0), stop=(c == 15))
                    return ins
                add("pe", f, reads=[("wr", slot)] + [("xnT", tb * 4 + i) for i in range(4)], writes=[("pG", pb)])
                return pb

            def evac_copy(pb, dst, dkeys, width=512, func=None):
                if func is not None:
                    add("act", lambda e: e.activation(out=dst, in_=pG[pb][:, 0:width], func=func),
                        reads=[("pG", pb)], writes=dkeys)
                    return
                ecnt[0] += 1
                if ecnt[0] % 2 == 0:
                    add("act", lambda e: e.activation(out=dst, in_=pG[pb][:, 0:width], func=AF.Copy),
                        reads=[("pG", pb)], writes=dkeys)
                else:
                    add("dve", lambda e: e.tensor_copy(out=dst, in_=pG[pb][:, 0:width]),
                        reads=[("pG", pb)], writes=dkeys)

            def block_F(c0, dst_d, row0, func=None):
                slot = load_w(c0, 512)
                for j in range(4):
                    sslot = scnt[0] % 2
                    scnt[0] += 1
                    for tb in range(4):
                        pb = gemm_F(slot, j, tb)
                        evac_copy(pb, stg[sslot][:, tb * 512:(tb + 1) * 512], [("stg", sslot, tb)], func=func)
                    add("sp", lambda e, j=j, sslot=sslot: e.dma_start(out=dst_d[row0 + j * 128:row0 + (j + 1) * 128, :], in_=stg[sslot][:]),
                        reads=[("stg", sslot, tb) for tb in range(4)], writes=[(id(dst_d), row0, j)], dma=True)

            def block_T(c0, dst_d, col0, kind):
                slot = load_w(c0, 512)
                hb = (col0 // 512) * 4
                for t4 in range(4):
                    sslot = scnt[0] % 2
                    scnt[0] += 1
                    for i in range(4):
                        t = t4 * 4 + i
                        pb = gemm_T(slot, t, 512)
                        dst = stg[sslot][:, i * 512:(i + 1) * 512]
                        dk_ = [("stg", sslot, i)]
                        if kind == "copy":
                            evac_copy(pb, dst, dk_)
                        elif kind == "silu":
                            evac_copy(pb, dst, dk_, func=AF.Silu)
                        else:
                            pv = pG[pb][:].rearrange("p (h two d) -> p h two d", h=4, two=2)
                            dv = dst.rearrange("p (h two d) -> p h two d", h=4, two=2)
                            cosb = cfs("cos")[:, t * 64:(t + 1) * 64].unsqueeze(1).to_broadcast([128, 4, 64])
                            sinb = cfs("sin")[:, t * 64:(t + 1) * 64].unsqueeze(1).to_broadcast([128, 4, 64])
                            rk = [("rtmp", q) for q in range(4)]
                            add("dve", lambda e, pv=pv, cosb=cosb: e.tensor_tensor(out=tmp[0][:], in0=pv[:, :, 0, :], in1=cosb, op=ALU.mult),
                                reads=[("pG", pb), "cf"], writes=[rk[0]])
                            add("dve", lambda e, pv=pv, sinb=sinb: e.tensor_tensor(out=tmp[1][:], in0=pv[:, :, 1, :], in1=sinb, op=ALU.mult),
                                reads=[("pG", pb), "cf"], writes=[rk[1]])
                            add("dve", lambda e, pv=pv, sinb=sinb: e.tensor_tensor(out=tmp[2][:], in0=pv[:, :, 0, :], in1=sinb, op=ALU.mult),
                                reads=[("pG", pb), "cf"], writes=[rk[2]])
                            add("dve", lambda e, pv=pv, cosb=cosb: e.tensor_tensor(out=tmp[3][:], in0=pv[:, :, 1, :], in1=cosb, op=ALU.mult),
                                reads=[("pG", pb), "cf"], writes=[rk[3]])
                            add("dve", lambda e, dv=dv: e.tensor_tensor(out=dv[:, :, 0, :], in0=tmp[0][:], in1=tmp[1][:], op=ALU.subtract),
                                reads=[rk[0], rk[1]], writes=dk_)
                            add("dve", lambda e, dv=dv: e.tensor_tensor(out=dv[:, :, 1, :], in0=tmp[2][:], in1=tmp[3][:], op=ALU.add),
                                reads=[rk[2], rk[3]], writes=dk_)
                            if kind == "rotk":
                                d2 = stg2[sslot][:, i * 512:(i + 1) * 512].rearrange("p (h d) -> p h d", h=4)
                                kd = cfs("kdec")[:, hb:hb + 4].unsqueeze(2).to_broadcast([128, 4, 128])
                                add("pool", lambda e, d2=d2, kd=kd, dst=dst: e.tensor_tensor(
                                    out=d2, in0=dst.rearrange("p (h d) -> p h d", h=4), in1=kd, op=ALU.mult),
                                    reads=dk_ + ["cf"], writes=[("stg2", sslot, i)])
                    dview = dst_d[t4 * 512:(t4 + 1) * 512, col0:col0 + 512].rearrange("(i p) n -> p i n", p=128)
                    add("sp", lambda e, dview=dview, sslot=sslot: e.dma_start(out=dview, in_=stg[sslot][:].rearrange("p (i n) -> p i n", i=4)),
                        reads=[("stg", sslot, i) for i in range(4)], writes=[(id(dst_d), col0, t4)], dma=True)
                    if kind == "rotk":
                        dview2 = kdec_d[t4 * 512:(t4 + 1) * 512, col0:col0 + 512].rearrange("(i p) n -> p i n", p=128)
                        add("sp", lambda e, dview2=dview2, sslot=sslot: e.dma_start(out=dview2, in_=stg2[sslot][:].rearrange("p (i n) -> p i n", i=4)),
                            reads=[("stg2", sslot, i) for i in range(4)], writes=[("kdec_d", col0, t4)], dma=True)

            slot = load_w(C_CKV, 128)
            ssk = sb(st, "ssk", [128, NT], F32)
            rsk = sb(st, "rsk", [128, NT], F32)
            junk2 = sb(st, "junk2", [128, 128], F32)
            for t in range(NT):
                pb = gemm_T(slot, t, 128)
                add("act", lambda e, t=t, pb=pb: e.activation(out=junk2[:], in_=pG[pb][:, 0:128], func=AF.Square, accum_out=ssk[:, t:t + 1]),
                    reads=[("pG", pb)], writes=["junk2", ("ssk", t)])
                add("act", lambda e, t=t: e.activation(out=rsk[:, t:t + 1], in_=ssk[:, t:t + 1], func=AF.Sqrt, scale=1.0 / 128, bias=EPS),
                    reads=[("ssk", t)], writes=[("rsk", t)])
                add("dve", lambda e, t=t: e.reciprocal(out=rsk[:, t:t + 1], in_=rsk[:, t:t + 1]),
                    reads=[("rsk", t)], writes=[("rsk", t)])
                add("dve", lambda e, t=t, pb=pb: e.scalar_tensor_tensor(out=kvtok[:, t, :], in0=pG[pb][:, 0:128], scalar=rsk[:, t:t + 1],
                                                                        in1=gkvbc[:], op0=ALU.mult, op1=ALU.mult),
                    reads=[("pG", pb), ("rsk", t), "gkvbc"], writes=[("kvtok", t)])
                add("pe", lambda e, t=t: e.transpose(out=pT[0][:, 0:128], in_=kvtok[:, t, :], identity=ident),
                    reads=[("kvtok", t), "cb"], writes=["pT0"])
                add("act", lambda e, t=t: e.activation(out=kvT[:, t * 128:(t + 1) * 128], in_=pT[0][:, 0:128], func=AF.Copy),
                    reads=["pT0"], writes=[("kvT", t)])
            slot = load_w(C_KIDX, 64, dup=True)
            for tb in range(4):
                pb = gemm_F(slot, 0, tb)
                evac_copy(pb, kidxT[:, tb * 512:(tb + 1) * 512], [("kidxT", tb)])
            slot = load_w(C_WIDX, 8)
            for t in range(NT):
                pb = gemm_T(slot, t, 8)
                add("act", lambda e, t=t, pb=pb: e.activation(out=wabs[:, t, :], in_=pG[pb][:, 0:8], func=AF.Abs),
                    reads=[("pG", pb)], writes=[("wabs", t)])
                add("dve", lambda e, t=t, pb=pb: e.tensor_scalar(out=wsgn[:, t, :], in0=pG[pb][:, 0:8], scalar1=0.0, scalar2=2.0,
                                                                 op0=ALU.is_ge, op1=ALU.mult),
                    reads=[("pG", pb)], writes=[("wsgn", t)])
                add("dve", lambda e, t=t: e.tensor_scalar(out=wsgn[:, t, :], in0=wsgn[:, t, :], scalar1=-1.0, scalar2=None, op0=ALU.add),
                    reads=[("wsgn", t)], writes=[("wsgn", t)])

            if upto >= 2:
                for i in range(2):
                    block_F(C_QLAT + i * 512, qlatT_d, i * 512)
                block_F(C_QIDX, qidxT_d, 0)
                for i in range(2):
                    block_T(C_QR + i * 512, qrot_d, i * 512, "rotq")
                for i in range(2):
                    block_T(C_KR + i * 512, krot_d, i * 512, "rotk")
                for i in range(2):
                    block_T(C_VR + i * 512, vr_d, i * 512, "copy")
                for i in range(2):
                    block_T(C_GR + i * 512, gr_d, i * 512, "silu")
                for i in range(8):
                    block_F(C_GBR + i * 512, gbT_d, i * 512, func=AF.Sigmoid)
            if debug:
                dump("kvT", kvT[:], [128, S], BF16, [("kvT", t) for t in range(NT)])
                dump("kvtok", kvtok[:], [128, NT, 128], BF16, [("kvtok", t) for t in range(NT)])
                dump("kidxT", kidxT[:], [128, S], BF16, [("kidxT", t) for t in range(4)])
                dump("wabs", wabs[:], [128, NT, 8], F32, [("wabs", t) for t in range(NT)])
                dump("wsgn", wsgn[:], [128, NT, 8], F32, [("wsgn", t) for t in range(NT)])
                dump("xnT", xnT[:], [128, NT, S], BF16, XN_ALL)
            sch.flush()

        if upto >= 3:
          with ExitStack() as st:
            NB = 6
            NIT = 16
            qlT = [sb(st, "qlT%d" % i, [128, 8, 128], BF16) for i in range(8)]
            qiT = [sb(st, "qiT%d" % i, [128, 4, 128], BF16) for i in range(8)]
            score = [sb(st, "score%d" % i, [128, S], F32) for i in range(4)]
            maskb = [sb(st, "maskb%d" % i, [128, S], BF16) for i in range(NB)]
            dsg = [sb(st, "dsg%d" % i, [128, 8, 128], BF16) for i in range(NB)]
            negw = [sb(st, "negw%d" % i, [128, S], F32) for i in range(2)]
            junkc = [sb(st, "junkc%d" % i, [128, S], BF16) for i in range(2)]
            rh = [sb(st, "rh%d" % i, [128, 512], BF16) for i in range(3)]
            m8a = [sb(st, "m8a%d" % i, [128, 8], F32) for i in range(NB)]
            m8n = [sb(st, "m8n%d" % i, [128, 8], F32) for i in range(NB)]
            lo = [sb(st, "lo%d" % i, [128, 1], F32) for i in range(NB)]
            stp = [sb(st, "stp%d" % i, [128, 1], F32) for i in range(NB)]
            mid = [sb(st, "mid%d" % i, [128, 1], F32) for i in range(NB)]
            cnt = [sb(st, "cnt%d" % i, [128, 1], F32) for i in range(NB)]
            tt = [sb(st, "tt%d" % i, [128, 1], F32) for i in range(NB)]
            thr0 = sb(st, "thr0", [128, 1], F32)
            PT = [sb(st, "PT%d" % i, [128, 512], BF16) for i in range(4)]
            rden = sb(st, "rden", [128, 1024], F32)
            oTs = sb(st, "oTs", [128, 1024], BF16)
            osb = [sb(st, "osb%d" % i, [128, 1024], F32) for i in range(4)]
            dsb = [sb(st, "dsb%d" % i, [128, 1024], F32) for i in range(4)]
            oaS = [sb(st, "oaS%d" % i, [128, 8, 128], BF16) for i in range(2)]
            wuv = sb(st, "wuv", [128, 8, 128], BF16)
            psI = [ps(st, "psI%d" % i, [128, 512], F32) for i in range(2)]
            psSc = [ps(st, "psSc%d" % i, [128, 512], F32) for i in range(2)]
            psS = [ps(st, "psS%d" % i, [128, 512], F32) for i in range(2)]
            psO = ps(st, "psO", [128, 512], F32)
            psD = ps(st, "psD", [128, 512], F32)
            ATT_SCALE = 128.0 ** -0.5
            add("pool", lambda e: e.dma_start(out=wuv[:], in_=w_uv.rearrange("h c d -> c h d")), writes=["wuv"], dma=True)
            add("dve", lambda e: e.memset(thr0[:], -1.0e29), writes=["thr0"])
            ident8 = cbs("ident8")
            ones_b = cbs("ones")
            icnt = [0]
            rcnt = [0]
            sccnt = [0]
            ptc = [0]
            scc = [0]

            def dsa_load(i):
                b = i % 8
                add("sp", lambda e: e.dma_start(out=qlT[b][:], in_=qlatT_d[:, i * 128:(i + 1) * 128].rearrange("(h c) q -> c h q", h=8)),
                    reads=[(id(qlatT_d), 0, j) for j in range(4)] + [(id(qlatT_d), 512, j) for j in range(4)],
                    writes=[("qlT", b)], dma=True)
                add("sp", lambda e: e.dma_start(out=qiT[b][:], in_=qidxT_d[:, i * 128:(i + 1) * 128].rearrange("(j r) q -> r j q", j=4)),
                    reads=[(id(qidxT_d), 0, j) for j in range(4)], writes=[("qiT", b)], dma=True)

            def dsa_index(i):
                b = i % NB
                nk = (i + 1) * 128
                nblk = (nk + 511) // 512
                add("dve", lambda e: e.tensor_tensor(out=dsg[b][:], in0=ident.unsqueeze(1).to_broadcast([128, 8, 128]),
                                                     in1=wsgn[:, i, :].unsqueeze(2).to_broadcast([128, 8, 128]), op=ALU.mult),
                    reads=["cb", ("wsgn", i)], writes=[("dsg", b)])
                seq = [(kb, h) for kb in range(nblk) for h in range(8)]
                info = {}

                def dots(idx):
                    kb, h = seq[idx]
                    width = min(512, nk - kb * 512)
                    pb = icnt[0] % 2
                    icnt[0] += 1
                    r = rcnt[0] % 3
                    rcnt[0] += 1
                    r0 = (h % 2) * 64
                    info[idx] = (pb, r, width)
                    add("pe", lambda e: e.matmul(psI[pb][:, 0:width], lhsT=qiT[i % 8][r0:r0 + 64, h // 2, :],
                                                 rhs=kidxT[r0:r0 + 64, kb * 512:kb * 512 + width], start=True, stop=True),
                        reads=[("qiT", i % 8)] + [("kidxT", q) for q in range(4)], writes=[("psI", pb)])
                    add("act", lambda e: e.activation(out=rh[r][:, 0:width], in_=psI[pb][:, 0:width], func=AF.Relu, scale=wabs[:, i, h:h + 1]),
                        reads=[("psI", pb), ("wabs", i)], writes=[("rh", r)])

                def acc(idx):
                    kb, h = seq[idx]
                    pb, r, width = info[idx]
                    if h == 0:
                        scc[0] = sccnt[0] % 2
                        sccnt[0] += 1
                    sc = scc[0]
                    add("pe", lambda e: e.matmul(psSc[sc][:, 0:width], lhsT=dsg[b][:, h, :], rhs=rh[r][:, 0:width], start=(h == 0), stop=(h == 7)),
                        reads=[("dsg", b), ("rh", r)], writes=[("psSc", sc)])
                    if h == 7:
                        add("act", lambda e: e.activation(out=score[i % 4][:, kb * 512:kb * 512 + width], in_=psSc[sc][:, 0:width], func=AF.Copy),
                            reads=[("psSc", sc)], writes=[("score", i % 4, kb)])
                dots(0)
                for idx in range(len(seq)):
                    if idx + 1 < len(seq):
                        dots(idx + 1)
                    acc(idx)
                    if idx % 2 == 1:
                        yield
                yield

            def dsa_topk(i):
                b = i % NB
                c = i % 2
                nk = (i + 1) * 128
                skeys = [("score", i % 4, kb) for kb in range((nk + 511) // 512)]
                add("dve", lambda e: e.memset(score[i % 4][0:64, nk - 64:nk], NEG),
                    reads=[("score", i % 4, (nk - 1) // 512)], writes=[("score", i % 4, (nk - 1) // 512)])
                if i < 2:
                    add("dve", lambda e: e.tensor_scalar(out=maskb[b][:, 0:nk], in0=score[i % 4][:, 0:nk], scalar1=thr0[:, 0:1], scalar2=-MBIG,
                                                         op0=ALU.is_lt, op1=ALU.mult),
                        reads=skeys + ["thr0"], writes=[("maskb", b)])
                    yield
                    return
                add("dve", lambda e: e.max(out=m8a[b][:], in_=score[i % 4][:, 0:nk]), reads=skeys, writes=[("m8a", b)])
                yield
                add("dve", lambda e: e.tensor_scalar(out=negw[c][:, 0:nk], in0=score[i % 4][:, 0:nk], scalar1=-1.0, scalar2=None, op0=ALU.mult),
                    reads=skeys, writes=[("negw", c)])
                yield
                add("dve", lambda e: e.memset(negw[c][0:64, nk - 64:nk], NEG), reads=[("negw", c)], writes=[("negw", c)])
                add("dve", lambda e: e.max(out=m8n[b][:], in_=negw[c][:, 0:nk]), reads=[("negw", c)], writes=[("m8n", b)])
                yield
                add("dve", lambda e: e.tensor_scalar(out=lo[b][:], in0=m8n[b][:, 0:1], scalar1=-1.0, scalar2=None, op0=ALU.mult),
                    reads=[("m8n", b)], writes=[("lo", b)])
                add("dve", lambda e: e.tensor_tensor(out=stp[b][:], in0=m8a[b][:, 0:1], in1=m8n[b][:, 0:1], op=ALU.add),
                    reads=[("m8a", b), ("m8n", b)], writes=[("stp", b)])
                yield
                add("dve", lambda e: e.tensor_scalar(out=stp[b][:], in0=stp[b][:], scalar1=0.5, scalar2=None, op0=ALU.mult),
                    reads=[("stp", b)], writes=[("stp", b)])
                yield
                for k in range(NIT):
                    add("dve", lambda e: e.tensor_tensor(out=mid[b][:], in0=lo[b][:], in1=stp[b][:], op=ALU.add),
                        reads=[("lo", b), ("stp", b)], writes=[("mid", b)])
                    yield
                    add("dve", lambda e: e.tensor_scalar(out=junkc[c][:, 0:nk], in0=score[i % 4][:, 0:nk], scalar1=mid[b][:, 0:1], scalar2=0.0,
                                                         op0=ALU.is_ge, op1=ALU.add, accum_out=cnt[b][:, 0:1]),
                        reads=skeys + [("mid", b)], writes=[("junkc", c), ("cnt", b)])
                    yield
                    add("dve", lambda e: e.tensor_scalar(out=tt[b][:], in0=cnt[b][:], scalar1=256.0, scalar2=stp[b][:, 0:1],
                                                         op0=ALU.is_ge, op1=ALU.mult),
                        reads=[("cnt", b), ("stp", b)], writes=[("tt", b)])
                    yield
                    add("dve", lambda e: e.tensor_tensor(out=lo[b][:], in0=lo[b][:], in1=tt[b][:], op=ALU.add),
                        reads=[("lo", b), ("tt", b)], writes=[("lo", b)])
                    add("dve", lambda e: e.tensor_scalar(out=stp[b][:], in0=stp[b][:], scalar1=0.5, scalar2=None, op0=ALU.mult),
                        reads=[("stp", b), ("tt", b)], writes=[("stp", b)])
                    yield
                add("dve", lambda e: e.tensor_scalar(out=maskb[b][:, 0:nk], in0=score[i % 4][:, 0:nk], scalar1=lo[b][:, 0:1], scalar2=-MBIG,
                                                     op0=ALU.is_lt, op1=ALU.mult),
                    reads=skeys + [("lo", b)], writes=[("maskb", b)])
                yield

            def dsa_attend(i):
                b = i % NB
                ob_ = i % 2
                for hf in range(2):
                    pis = {}

                    def fS_add(kt, hf=hf):
                        def fS(e):
                            e.matmul(psS[kt % 2][:, :], lhsT=kvT[:, kt * 128:(kt + 1) * 128], rhs=qlT[i % 8][:, hf * 4:(hf + 1) * 4, :],
                                     start=True, stop=False)
                            return e.matmul(psS[kt % 2][:, :], lhsT=maskb[b][:, kt * 128:(kt + 1) * 128], rhs=ident8[:, 0:512],
                                            start=False, stop=True)
                        add("pe", fS, reads=[("kvT", kt), ("qlT", i % 8), ("maskb", b), "cb"], writes=[("psS", kt % 2)])
                    fS_add(0)
                    for kt in range(i + 1):
                        if kt + 1 <= i:
                            fS_add(kt + 1)
                        pi = ptc[0] % 4
                        ptc[0] += 1
                        add("act", lambda e, kt=kt, pi=pi: e.activation(out=PT[pi][:], in_=psS[kt % 2][:, :], func=AF.Exp, scale=ATT_SCALE),
                            reads=[("psS", kt % 2)], writes=[("PT", pi)])

                        def fO(e, kt=kt, pi=pi):
                            e.matmul(psO[:, :], lhsT=kvtok[:, kt, :], rhs=PT[pi][:], start=(kt == 0), stop=(kt == i))
                            return e.matmul(psD[:, :], lhsT=ones_b, rhs=PT[pi][:], start=(kt == 0), stop=(kt == i))
                        add("pe", fO, reads=[("kvtok", kt), ("PT", pi), "cb"], writes=["psO", "psD"])
                    add("act", lambda e, hf=hf: e.activation(out=dsb[i % 4][:, hf * 512:(hf + 1) * 512], in_=psD[:, :], func=AF.Copy),
                        reads=["psD"], writes=[("dsb", i % 4, hf)])
                    add("act", lambda e, hf=hf: e.activation(out=osb[i % 4][:, hf * 512:(hf + 1) * 512], in_=psO[:, :], func=AF.Copy),
                        reads=["psO"], writes=[("osb", i % 4, hf)])
                yield

            def dsa_finish(i):
                b = i % NB
                ob_ = i % 2
                for hf in range(2):
                    add("dve", lambda e, hf=hf: e.reciprocal(out=rden[:, hf * 512:(hf + 1) * 512], in_=dsb[i % 4][:, hf * 512:(hf + 1) * 512]),
                        reads=[("dsb", i % 4, hf)], writes=[("rden", hf)])
                    add("dve", lambda e, hf=hf: e.tensor_tensor(out=oTs[:, hf * 512:(hf + 1) * 512], in0=osb[i % 4][:, hf * 512:(hf + 1) * 512],
                                                                in1=rden[:, hf * 512:(hf + 1) * 512], op=ALU.mult),
                        reads=[("osb", i % 4, hf), ("rden", hf)], writes=[("oTs", hf)])
                for hf in range(2):
                    def fU(e, hf=hf):
                        ins = None
                        for hh in range(4):
                            h = hf * 4 + hh
                            ins = e.matmul(psI[hf][:, hh * 128:(hh + 1) * 128], lhsT=wuv[:, h, :], rhs=oTs[:, h * 128:(h + 1) * 128],
                                           start=True, stop=True)
                        return ins
                    add("pe", fU, reads=["wuv", ("oTs", hf)], writes=[("psI", hf)])
                    add("act", lambda e, hf=hf: e.activation(out=oaS[ob_][:, hf * 4:(hf + 1) * 4, :],
                                                             in_=psI[hf][:, :].rearrange("p (h q) -> p h q", h=4), func=AF.Copy),
                        reads=[("psI", hf)], writes=[("oaS", ob_, hf)])
                add("sp", lambda e: e.dma_start(out=oaT_d[:, i * 128:(i + 1) * 128].rearrange("(h d) q -> d h q", h=8), in_=oaS[ob_][:]),
                    reads=[("oaS", ob_, 0), ("oaS", ob_, 1)], writes=[("oaT_d", i)], dma=True)
                yield

            def chain(*gens):
                for g in gens:
                    yield from g

            NTD = int(os.environ.get("DSA_TILES", NT))
            NPAIR = NTD // 2
            for step in range(NPAIR + 3):
                pi_ = step
                pt_ = step - 1
                pa_ = step - 2
                pf_ = step - 3
                conv(2)
                if step == 0:
                    dsa_load(0)
                    dsa_load(1)
                if pi_ + 1 < NPAIR:
                    dsa_load(2 * pi_ + 2)
                    dsa_load(2 * pi_ + 3)
                if 0 <= pf_ < NPAIR:
                    for i in (2 * pf_, 2 * pf_ + 1):
                        for _ in dsa_finish(i):
                            pass
                if pi_ < NPAIR:
                    for i in (2 * pi_, 2 * pi_ + 1):
                        for _ in dsa_index(i):
                            pass
                if 0 <= pt_ < NPAIR:
                    gens = [dsa_topk(2 * pt_), dsa_topk(2 * pt_ + 1)]
                    while gens:
                        for g in list(gens):
                            try:
                                next(g)
                            except StopIteration:
                                gens.remove(g)
                if 0 <= pa_ < NPAIR:
                    for i in (2 * pa_, 2 * pa_ + 1):
                        for _ in dsa_attend(i):
                            pass
            sch.flush()

        KC.close()
        if upto >= 4:
          with ExitStack() as st:
            qr = [sb(st, "qr%d" % i, [128, 1024], BF16) for i in range(2)]
            kr = [sb(st, "kr%d" % i, [128, 1024], BF16) for i in range(2)]
            kd = [sb(st, "kd%d" % i, [128, 1024], BF16) for i in range(2)]
            vr = [sb(st, "vr%d" % i, [128, 1024], BF16) for i in range(2)]
            gr = [sb(st, "gr%d" % i, [128, 1024], BF16) for i in range(2)]
            qdT = sb(st, "qdT", [128, 1024], BF16)
            kTs = sb(st, "kTs", [128, 1024], BF16)
            ATs = sb(st, "ATs", [128, 1024], BF16)
            gkm = sb(st, "gkm", [128, 8, 128], F32)
            Sf = sb(st, "Sf", [128, 1024], F32)
            Sb = sb(st, "Sb", [128, 1024], BF16)
            osq = sb(st, "osq", [128, 1024], F32)
            oc = sb(st, "oc", [128, 1024], F32)
            ob = sb(st, "ob", [128, 1024], BF16)
            obS = [sb(st, "obS%d" % i, [128, 8, 128], BF16) for i in range(2)]
            gretbc = sb(st, "gretbc", [128, 1024], F32)
            st8 = sb(st, "st8", [128, 8], F32)
            sq8 = sb(st, "sq8", [128, 8], F32)
            mean8 = sb(st, "mean8", [128, 8], F32)
            var8 = sb(st, "var8", [128, 8], F32)
            rs8 = sb(st, "rs8", [128, 8], F32)
            pTq = ps(st, "pTq", [128, 1024], BF16)
            pTk = ps(st, "pTk", [128, 1024], BF16)
            psA = [ps(st, "psA%d" % i, [128, 512], F32) for i in range(2)]
            psR = [ps(st, "psR%d" % i, [128, 512], F32) for i in range(2)]
            psT = [ps(st, "psT%d" % i, [128, 512], F32) for i in range(2)]
            add("sp", lambda e: e.dma_start(out=gretbc[:], in_=g_ret[0:1, :].to_broadcast([128, 1024])), writes=["gretbc"], dma=True)
            for h in range(8):
                add("dve", lambda e, h=h: e.tensor_scalar(out=gkm[:, h, :], in0=cfs("cmask"), scalar1=cfs("gk")[:, h:h + 1], scalar2=None, op0=ALU.mult),
                    reads=["cf"], writes=["gkm"])
            add("dve", lambda e: e.memset(Sf[:], 0.0), writes=["Sf"])
            dqt = cfs("dq")

            def ret_load(t):
                b = t % 2
                rows = slice(t * 128, (t + 1) * 128)
                for name, dst, src in (("qr", qr, qrot_d), ("kr", kr, krot_d), ("kd", kd, kdec_d), ("vr", vr, vr_d), ("gr", gr, gr_d)):
                    rk = [("kdec_d", c0, t // 4) for c0 in (0, 512)] if name == "kd" else [(id(src), c0, t // 4) for c0 in (0, 512)]
                    add("sp", lambda e, dst=dst, src=src: e.dma_start(out=dst[b][:], in_=src[rows, :]), reads=rk, writes=[(name, b)], dma=True)

            NTR = int(os.environ.get("RET_TILES", NT))
            RP = int(os.environ.get("RET_PART", 9))
            ret_load(0)
            for t in range(NTR):
                b = t % 2
                if t + 1 < NTR:
                    ret_load(t + 1)

                def fT(e, b=b):
                    ins = None
                    for h in range(8):
                        e.transpose(out=pTq[:, h * 128:(h + 1) * 128], in_=qr[b][:, h * 128:(h + 1) * 128], identity=ident)
                        ins = e.transpose(out=pTk[:, h * 128:(h + 1) * 128], in_=kr[b][:, h * 128:(h + 1) * 128], identity=ident)
                    return ins
                add("pe", fT, reads=[("qr", b), ("kr", b), "cb"], writes=["pTq", "pTk"])
                add("dve", lambda e: e.tensor_tensor(out=qdT[:], in0=pTq[:, :], in1=dqt, op=ALU.mult), reads=["pTq", "cf"], writes=["qdT"])
                add("act", lambda e: e.activation(out=kTs[:], in_=pTk[:, :], func=AF.Copy), reads=["pTk"], writes=["kTs"])
                for hf in range(2):
                    def fA(e, hf=hf):
                        ins = None
                        for hh in range(4):
                            h = hf * 4 + hh
                            ins = e.matmul(psA[hf][:, hh * 128:(hh + 1) * 128], lhsT=kTs[:, h * 128:(h + 1) * 128], rhs=qdT[:, h * 128:(h + 1) * 128],
                                           start=True, stop=True)
                        return ins
                    add("pe", fA, reads=["kTs", "qdT"], writes=[("psA", hf)])
                    add("dve", lambda e, hf=hf: e.tensor_tensor(out=ATs[:, hf * 512:(hf + 1) * 512], in0=psA[hf][:, :],
                                                                in1=gkm[:, hf * 4:(hf + 1) * 4, :].rearrange("p h n -> p (h n)"), op=ALU.mult),
                        reads=[("psA", hf), "gkm"], writes=[("ATs", hf)])
                for hf in range(2):
                    def fR(e, hf=hf, t=t, b=b):
                        ins = None
                        for hh in range(4):
                            h = hf * 4 + hh
                            ins = e.matmul(psR[hf][:, hh * 128:(hh + 1) * 128], lhsT=ATs[:, h * 128:(h + 1) * 128], rhs=vr[b][:, h * 128:(h + 1) * 128],
                                           start=True, stop=(t == 0))
                            if t > 0:
                                ins = e.matmul(psR[hf][:, hh * 128:(hh + 1) * 128], lhsT=qdT[:, h * 128:(h + 1) * 128], rhs=Sb[:, h * 128:(h + 1) * 128],
                                               start=False, stop=True)
                        return ins
                    add("pe", fR, reads=[("ATs", hf), ("vr", b), "qdT", "Sb"], writes=[("psR", hf)])
                for hf in range(2):
                    def fS2(e, hf=hf, b=b):
                        ins = None
                        for hh in range(4):
                            h = hf * 4 + hh
                            ins = e.matmul(psT[hf][:, hh * 128:(hh + 1) * 128], lhsT=kd[b][:, h * 128:(h + 1) * 128], rhs=vr[b][:, h * 128:(h + 1) * 128],
                                           start=True, stop=True)
                        return ins
                    add("pe", fS2, reads=[("kd", b), ("vr", b)], writes=[("psT", hf)])
                if RP < 2:
                    continue
                for h in range(8):
                    hsl = slice(h * 128, (h + 1) * 128)
                    psl = psR[h // 4][:, (h % 4) * 128:(h % 4 + 1) * 128]
                    add("act", lambda e, h=h, hsl=hsl, psl=psl: e.activation(out=oc[:, hsl], in_=psl, func=AF.Copy, accum_out=st8[:, h:h + 1]),
                        reads=[("psR", h // 4)], writes=[("oc", h // 4), ("st8", h // 4)])
                    add("act", lambda e, h=h, hsl=hsl, psl=psl: e.activation(out=osq[:, hsl], in_=psl, func=AF.Square, accum_out=sq8[:, h:h + 1]),
                        reads=[("psR", h // 4)], writes=[("osq", h // 4), ("sq8", h // 4)])
                if RP < 3:
                    continue
                add("dve", lambda e: e.tensor_scalar(out=mean8[:], in0=st8[:], scalar1=1.0 / 128, scalar2=None, op0=ALU.mult),
                    reads=[("st8", 0), ("st8", 1)], writes=["mean8"])
                add("dve", lambda e: e.tensor_tensor(out=var8[:], in0=mean8[:], in1=mean8[:], op=ALU.mult), reads=["mean8"], writes=["var8"])
                add("dve", lambda e: e.scalar_tensor_tensor(out=var8[:], in0=sq8[:], scalar=1.0 / 128, in1=var8[:], op0=ALU.mult, op1=ALU.subtract),
                    reads=[("sq8", 0), ("sq8", 1), "var8"], writes=["var8"])
                add("act", lambda e: e.activation(out=rs8[:], in_=var8[:], func=AF.Sqrt, scale=1.0, bias=EPS), reads=["var8"], writes=["rs8"])
                add("dve", lambda e: e.reciprocal(out=rs8[:], in_=rs8[:]), reads=["rs8"], writes=["rs8"])
                for hf in range(2):
                    ocv = oc[:, hf * 512:(hf + 1) * 512].rearrange("p (h n) -> p h n", h=4)
                    add("dve", lambda e, hf=hf, ocv=ocv: e.tensor_tensor(out=ocv, in0=ocv,
                                                                         in1=mean8[:, hf * 4:(hf + 1) * 4].unsqueeze(2).to_broadcast([128, 4, 128]), op=ALU.subtract),
                        reads=[("oc", hf), "mean8"], writes=[("oc", hf)])
                    add("dve", lambda e, hf=hf, ocv=ocv: e.tensor_tensor(out=ocv, in0=ocv,
                                                                         in1=rs8[:, hf * 4:(hf + 1) * 4].unsqueeze(2).to_broadcast([128, 4, 128]), op=ALU.mult),
                        reads=[("oc", hf), "rs8"], writes=[("oc", hf)])
                if RP < 4:
                    continue
                add("pool", lambda e: e.tensor_tensor(out=oc[:], in0=oc[:], in1=gretbc[:], op=ALU.mult),
                    reads=[("oc", 0), ("oc", 1), "gretbc"], writes=[("oc", 0), ("oc", 1)])
                add("dve", lambda e, b=b: e.tensor_tensor(out=ob[:], in0=oc[:], in1=gr[b][:], op=ALU.mult),
                    reads=[("oc", 0), ("oc", 1), ("gr", b)], writes=["ob"])
                if RP < 5:
                    continue
                for h in range(8):
                    add("dve", lambda e, h=h: e.scalar_tensor_tensor(out=Sf[:, h * 128:(h + 1) * 128], in0=Sf[:, h * 128:(h + 1) * 128], scalar=_G128[h],
                                                                     in1=psT[h // 4][:, (h % 4) * 128:(h % 4 + 1) * 128], op0=ALU.mult, op1=ALU.add),
                        reads=["Sf", ("psT", h // 4)], writes=["Sf"])
                add("act", lambda e: e.activation(out=Sb[:], in_=Sf[:], func=AF.Copy), reads=["Sf"], writes=["Sb"])

                def fT2(e):
                    ins = None
                    for h in range(8):
                        ins = e.transpose(out=pTq[:, h * 128:(h + 1) * 128], in_=ob[:, h * 128:(h + 1) * 128], identity=ident)
                    return ins
                add("pe", fT2, reads=["ob", "cb"], writes=["pTq"])
                add("act", lambda e, b=b: e.activation(out=obS[b][:], in_=pTq[:, :].rearrange("p (h q) -> p h q", h=8), func=AF.Copy),
                    reads=["pTq"], writes=[("obS", b)])
                add("sp", lambda e, t=t, b=b: e.dma_start(out=obT_d[:, t * 128:(t + 1) * 128].rearrange("(h d) q -> d h q", h=8), in_=obS[b][:]),
                    reads=[("obS", b)], writes=[("obT_d", t)], dma=True)
            sch.flush()

        lg = sb(P, "lg", [128, NT, 36], F32)
        d1i = sb(P, "d1i", [128, NT], I32)
        d2i = sb(P, "d2i", [128, NT], I32)
        g1 = sb(P, "g1", [128, NT], F32)
        g2 = sb(P, "g2", [128, NT], F32)

        if upto >= 5:
          with ExitStack() as st2:
            mixT = sb(st2, "mixT", [128, 16, S], BF16)
            with ExitStack() as st:
                Wb = sb(st, "Wb", [128, 2, 8, D], BF16)
                oaB = sb(st, "oaB", [128, 8, 512], BF16)
                obB = sb(st, "obB", [128, 8, 512], BF16)
                gAB = [sb(st, "gAB%d" % i, [128, 2, 512], BF16) for i in range(3)]
                t1 = [sb(st, "t1_%d" % i, [128, 512], F32) for i in range(2)]
                t2 = [sb(st, "t2_%d" % i, [128, 512], F32) for i in range(2)]
                psB = [ps(st, "psB%d" % i, [128, 512], F32) for i in range(4)]
                for hc in range(2):
                    for n in range(2):
                        add("pool", lambda e, n=n, hc=hc: e.dma_start(
                            out=Wb[:, n, :, hc * 1024:(hc + 1) * 1024],
                            in_=w_branch[n, :, hc * 1024:(hc + 1) * 1024].rearrange("(c p) d -> p c d", p=128)),
                            writes=[("Wb", n, hc)], dma=True)
                WBK = [("Wb", n, hc) for n in range(2) for hc in range(2)]
                gc = 0
                for tb in range(4):
                    cols = slice(tb * 512, (tb + 1) * 512)
                    add("sp", lambda e, cols=cols: e.dma_start(out=oaB[:], in_=oaT_d[:, cols].rearrange("(c p) q -> p c q", p=128)),
                        reads=[("oaT_d", i) for i in range(tb * 4, tb * 4 + 4)], writes=["oaB"], dma=True)
                    add("sp", lambda e, cols=cols: e.dma_start(out=obB[:], in_=obT_d[:, cols].rearrange("(c p) q -> p c q", p=128)),
                        reads=[("obT_d", i) for i in range(tb * 4, tb * 4 + 4)], writes=["obB"], dma=True)
                    for j in range(16):
                        gs = gc % 3
                        pa = (gc % 2) * 2
                        gc += 1
                        add("sp", lambda e, j=j, gs=gs, cols=cols: e.dma_start(
                            out=gAB[gs][:], in_=gbT_d.rearrange("(n r) q -> r n q", n=2)[j * 128:(j + 1) * 128, :, cols]),
                            reads=[(id(gbT_d), (j // 4) * 512, j % 4), (id(gbT_d), 2048 + (j // 4) * 512, j % 4)], writes=[("gAB", gs)], dma=True)
                        for n, src in ((0, oaB), (1, obB)):
                            def fB(e, n=n, src=src, j=j, pa=pa):
                                ins = None
                                for c in range(8):
                                    ins = e.matmul(psB[pa + n][:, :], lhsT=Wb[:, n, c, j * 128:(j + 1) * 128], rhs=src[:, c, :],
                                                   start=(c == 0), stop=(c == 7))
                                return ins
                            add("pe", fB, reads=[("Wb", n, j // 8), "oaB" if n == 0 else "obB"], writes=[("psB", pa + n)])
                        q = (gc % 2)
                        add("dve", lambda e, gs=gs, pa=pa, q=q: e.tensor_tensor(out=t1[q][:], in0=psB[pa][:, :], in1=gAB[gs][:, 0, :], op=ALU.mult),
                            reads=[("psB", pa), ("gAB", gs)], writes=[("t1", q)])
                        add("dve", lambda e, gs=gs, pa=pa, q=q: e.tensor_tensor(out=t2[q][:], in0=psB[pa + 1][:, :], in1=gAB[gs][:, 1, :], op=ALU.mult),
                            reads=[("psB", pa + 1), ("gAB", gs)], writes=[("t2", q)])
                        add("pool", lambda e, j=j, q=q, cols=cols: e.tensor_tensor(out=mixT[:, j, cols], in0=t1[q][:], in1=t2[q][:], op=ALU.add),
                            reads=[("t1", q), ("t2", q)], writes=[("mixT", tb)])
                sch.flush()

            with ExitStack() as st:
                Wo = sb(st, "Wo", [128, 16, D], BF16)
                Wr = sb(st, "Wr", [128, 16, 36], BF16)
                brbc = sb(st, "brbc", [128, 36], F32)
                gfbc = sb(st, "gfbc", [128, D], F32)
                xt2 = [sb(st, "xt2_%d" % i, [128, D], F32) for i in range(2)]
                h1t = [sb(st, "h1t%d" % i, [128, D], F32) for i in range(2)]
                xn2 = [sb(st, "xn2_%d" % i, [128, D], BF16) for i in range(2)]
                xn2T = sb(st, "xn2T", [128, 16, 128], BF16)
                junk3 = sb(st, "junk3", [128, D], BF16)
                ss2 = sb(st, "ss2", [128, NT], F32)
                rs2 = sb(st, "rs2", [128, NT], F32)
                psH = [ps(st, "psH%d" % i, [128, 512], F32) for i in range(4)]
                pT2 = [ps(st, "pT2_%d" % i, [128, 1024], BF16) for i in range(2)]
                psL = ps(st, "psL", [128, 512], F32)
                for hc in range(4):
                    add("pool", lambda e, hc=hc: e.dma_start(out=Wo[:, :, hc * 512:(hc + 1) * 512],
                                                             in_=w_out[:, hc * 512:(hc + 1) * 512].rearrange("(c p) d -> p c d", p=128)),
                        writes=[("Wo", hc)], dma=True)
                add("pool", lambda e: e.dma_start(out=Wr[:, :, 0:4], in_=w_rg.rearrange("(c p) g -> p c g", p=128)), writes=["Wr"], dma=True)
                add("pool", lambda e: e.dma_start(out=Wr[:, :, 4:36], in_=w_re.rearrange("(c p) g -> p c g", p=128)), writes=["Wr"], dma=True)
                add("sp", lambda e: e.dma_start(out=brbc[:, 0:4], in_=b_rg[0:1, :].to_broadcast([128, 4])), writes=["brbc"], dma=True)
                add("sp", lambda e: e.dma_start(out=brbc[:, 4:36], in_=b_re[0:1, :].to_broadcast([128, 32])), writes=["brbc"], dma=True)
                add("sp", lambda e: e.dma_start(out=gfbc[:], in_=g_ffn[0:1, :].to_broadcast([128, D])), writes=["gfbc"], dma=True)
                hc_ = [0]

                def e2_front(t):
                    b = t % 2
                    rows = slice(t * 128, (t + 1) * 128)
                    add("sp", lambda e, b=b, rows=rows: e.dma_start(out=xt2[b][:], in_=x[rows, :]), writes=[("xt2", b)], dma=True)
                    for cbk in range(4):
                        pb = hc_[0] % 4
                        hc_[0] += 1

                        def fH(e, t=t, cbk=cbk, pb=pb):
                            ins = None
                            for j in range(16):
                                ins = e.matmul(psH[pb][:, :], lhsT=mixT[:, j, t * 128:(t + 1) * 128], rhs=Wo[:, j, cbk * 512:(cbk + 1) * 512],
                                               start=(j == 0), stop=(j == 15))
                            return ins
                        add("pe", fH, reads=[("mixT", t // 4), ("Wo", cbk)], writes=[("psH", pb)])
                        add("dve", lambda e, b=b, cbk=cbk, pb=pb: e.tensor_tensor(out=h1t[b][:, cbk * 512:(cbk + 1) * 512], in0=psH[pb][:, :],
                                                                                  in1=xt2[b][:, cbk * 512:(cbk + 1) * 512], op=ALU.add),
                            reads=[("psH", pb), ("xt2", b)], writes=[("h1t", b, cbk)])

                def e2_back(t):
                    b = t % 2
                    rows = slice(t * 128, (t + 1) * 128)
                    conv(1)
                    HK = [("h1t", b, c) for c in range(4)]
                    add("sp", lambda e, b=b, rows=rows: e.dma_start(out=h1_d[rows, :], in_=h1t[b][:]), reads=HK, writes=[("h1_d", t)], dma=True)
                    add("act", lambda e, t=t, b=b: e.activation(out=junk3[:], in_=h1t[b][:], func=AF.Square, accum_out=ss2[:, t:t + 1]),
                        reads=HK, writes=["junk3", ("ss2", t)])
                    add("act", lambda e, t=t: e.activation(out=rs2[:, t:t + 1], in_=ss2[:, t:t + 1], func=AF.Sqrt, scale=1.0 / D, bias=EPS),
                        reads=[("ss2", t)], writes=[("rs2", t)])
                    add("dve", lambda e, t=t: e.reciprocal(out=rs2[:, t:t + 1], in_=rs2[:, t:t + 1]), reads=[("rs2", t)], writes=[("rs2", t)])
                    add("dve", lambda e, t=t, b=b: e.scalar_tensor_tensor(out=xn2[b][:], in0=h1t[b][:], scalar=rs2[:, t:t + 1], in1=gfbc[:],
                                                                          op0=ALU.mult, op1=ALU.mult),
                        reads=HK + [("rs2", t), "gfbc"], writes=[("xn2", b)])
                    add("sp", lambda e, b=b, rows=rows: e.dma_start(out=xn2_d[rows, :], in_=xn2[b][:]), reads=[("xn2", b)], writes=[("xn2_d", t)], dma=True)

                    def tr2(e, b=b):
                        ins = None
                        for c in range(16):
                            ins = e.transpose(out=pT2[c // 8][:, (c % 8) * 128:(c % 8 + 1) * 128], in_=xn2[b][:, c * 128:(c + 1) * 128], identity=ident)
                        return ins
                    add("pe", tr2, reads=[("xn2", b), "cb"], writes=["pT2a", "pT2b"])
                    add("act", lambda e: e.activation(out=xn2T[:, 0:8, :], in_=pT2[0][:].rearrange("p (c n) -> p c n", c=8), func=AF.Copy),
                        reads=["pT2a"], writes=["xn2Ta"])
                    add("dve", lambda e: e.tensor_copy(out=xn2T[:, 8:16, :], in_=pT2[1][:].rearrange("p (c n) -> p c n", c=8)),
                        reads=["pT2b"], writes=["xn2Tb"])

                    def fL(e):
                        ins = None
                        for c in range(16):
                            ins = e.matmul(psL[:, 0:36], lhsT=xn2T[:, c, :], rhs=Wr[:, c, :], start=(c == 0), stop=(c == 15))
                        return ins
                    add("pe", fL, reads=["xn2Ta", "xn2Tb", "Wr"], writes=["psL"])
                    add("dve", lambda e, t=t: e.tensor_tensor(out=lg[:, t, :], in0=psL[:, 0:36], in1=brbc[:], op=ALU.add),
                        reads=["psL", "brbc"], writes=[("lg", t)])

                e2_front(0)
                for t in range(NT):
                    if t + 1 < NT:
                        e2_front(t + 1)
                    e2_back(t)
                sch.flush()

        GW = ExitStack()
        NU = 6
        wu = [sb(GW, "wu%d" % i, [128, 16 * 512], BF16) for i in range(NU)]
        uc = [0]

        def load_unit(src, is_down, half, rd=()):
            u = uc[0] % NU
            uc[0] += 1
            if not is_down:
                add("pool", lambda e: e.dma_start(out=wu[u][:].rearrange("p (c f) -> p c f", c=16),
                                                  in_=src[:, half * 512:(half + 1) * 512].rearrange("(c p) f -> p c f", p=128)),
                    reads=list(rd), writes=[("wu", u)], dma=True)
            else:
                add("pool", lambda e: e.dma_start(out=wu[u][:].rearrange("p (c f) -> p c f", c=8),
                                                  in_=src[:, half * 1024:(half + 1) * 1024].rearrange("(c p) f -> p c f", p=128)),
                    reads=list(rd), writes=[("wu", u)], dma=True)
            return u

        conv(1000)
        preloaded = {}
        if upto >= 7:
            for key, src, dn, half in (("g0", w_eg[0], False, 0), ("u0", w_eu[0], False, 0), ("g1", w_eg[0], False, 1),
                                       ("u1", w_eu[0], False, 1), ("d0", w_ed[0], True, 0), ("d1", w_ed[0], True, 1)):
                preloaded[(0, key)] = load_unit(src, dn, half)

        def get_unit(ex, key, src, dn, half):
            if (ex, key) in preloaded:
                return preloaded.pop((ex, key))
            if ex >= NF32:
                kind = key[0]
                src = {"g": wgb_d, "u": wub_d, "d": wdb_d}[kind][ex - NF32]
                return load_unit(src, dn, half, rd=[("wconv", ex, kind)])
            return load_unit(src, dn, half)

        if upto >= 6:
          with ExitStack() as st:
            gmax = sb(st, "gmax", [128, NT], F32)
            eg = sb(st, "eg", [128, NT, 4], F32)
            gsum = sb(st, "gsum", [128, NT], F32)
            pgrp = sb(st, "pgrp", [128, NT], F32)
            pen = sb(st, "pen", [128, NT, 4], F32)
            lm = sb(st, "lm", [128, NT, 32], F32)
            m8r = sb(st, "m8r", [128, NT, 8], F32)
            oh1 = sb(st, "oh1", [128, NT, 32], F32)
            oh2 = sb(st, "oh2", [128, NT, 32], F32)
            ohb = sb(st, "ohb", [128, NT, 32], BF16)
            dl = sb(st, "dl", [128, NT], F32)
            sg = sb(st, "sg", [128, NT], F32)
            tmpc = sb(st, "tmpc", [128, NT, 32], F32)
            tmpd = sb(st, "tmpd", [128, NT, 32], F32)
            d1f = sb(st, "d1f", [128, NT], F32)
            d2f = sb(st, "d2f", [128, NT], F32)
            xg = [sb(st, "xg%d" % i, [128, D], BF16) for i in range(2)]
            psC = ps(st, "psC", [128, 512], F32)
            LG = [("lg", t) for t in range(NT)]
            add("dve", lambda e: e.tensor_tensor(out=eg[:, :, 0:2], in0=lg[:, :, 0:2], in1=lg[:, :, 2:4], op=ALU.max), reads=LG, writes=["eg"])
            add("dve", lambda e: e.tensor_tensor(out=gmax[:].unsqueeze(2), in0=eg[:, :, 0:1], in1=eg[:, :, 1:2], op=ALU.max), reads=["eg"], writes=["gmax"])
            add("dve", lambda e: e.tensor_tensor(out=eg[:], in0=lg[:, :, 0:4], in1=gmax[:].unsqueeze(2).to_broadcast([128, NT, 4]), op=ALU.subtract),
                reads=LG + ["gmax"], writes=["eg"])
            add("dve", lambda e: e.tensor_scalar(out=pen[:], in0=eg[:], scalar1=0.0, scalar2=-1.0e30, op0=ALU.is_lt, op1=ALU.mult),
                reads=["eg"], writes=["pen"])
            add("act", lambda e: e.activation(out=eg[:], in_=eg[:], func=AF.Exp), reads=["eg"], writes=["eg"])
            add("dve", lambda e: e.tensor_tensor(out=pgrp[:].unsqueeze(2), in0=eg[:, :, 0:1], in1=eg[:, :, 1:2], op=ALU.add), reads=["eg"], writes=["pgrp"])
            add("dve", lambda e: e.tensor_tensor(out=gsum[:].unsqueeze(2), in0=eg[:, :, 2:3], in1=eg[:, :, 3:4], op=ALU.add), reads=["eg"], writes=["gsum"])
            add("dve", lambda e: e.tensor_tensor(out=gsum[:], in0=gsum[:], in1=pgrp[:], op=ALU.add), reads=["gsum", "pgrp"], writes=["gsum"])
            add("dve", lambda e: e.reciprocal(out=pgrp[:], in_=gsum[:]), reads=["gsum"], writes=["pgrp"])
            add("dve", lambda e: e.tensor_tensor(out=lm[:].rearrange("p t (g k) -> p t g k", g=4),
                                                 in0=lg[:, :, 4:36].rearrange("p t (g k) -> p t g k", g=4),
                                                 in1=pen[:].unsqueeze(3).to_broadcast([128, NT, 4, 8]), op=ALU.add),
                reads=LG + ["pen"], writes=["lm"])
            for t in range(NT):
                add("dve", lambda e, t=t: e.max(out=m8r[:, t, :], in_=lm[:, t, :]), reads=["lm"], writes=["m8r"])
            add("dve", lambda e: e.tensor_tensor(out=oh1[:], in0=lm[:], in1=m8r[:, :, 0:1].to_broadcast([128, NT, 32]), op=ALU.is_equal),
                reads=["lm", "m8r"], writes=["oh1"])
            add("dve", lambda e: e.tensor_tensor(out=oh2[:], in0=lm[:], in1=m8r[:, :, 1:2].to_broadcast([128, NT, 32]), op=ALU.is_equal),
                reads=["lm", "m8r"], writes=["oh2"])
            add("dve", lambda e: e.tensor_tensor(out=ohb[:], in0=oh1[:], in1=oh2[:], op=ALU.add), reads=["oh1", "oh2"], writes=["ohb"])
            add("dve", lambda e: e.tensor_tensor(out=dl[:].unsqueeze(2), in0=m8r[:, :, 0:1], in1=m8r[:, :, 1:2], op=ALU.subtract), reads=["m8r"], writes=["dl"])
            add("act", lambda e: e.activation(out=sg[:], in_=dl[:], func=AF.Sigmoid), reads=["dl"], writes=["sg"])
            add("dve", lambda e: e.tensor_tensor(out=g1[:], in0=sg[:], in1=pgrp[:], op=ALU.mult), reads=["sg", "pgrp"], writes=["g1"])
            add("dve", lambda e: e.tensor_tensor(out=g2[:], in0=pgrp[:], in1=g1[:], op=ALU.subtract), reads=["g1", "pgrp"], writes=["g2"])
            ones_b = cbs("ones")
            utri = cbs("utri")
            for t in range(NT):
                def fC(e, t=t):
                    ins = None
                    for tp in range(t):
                        ins = e.matmul(psC[:, t * 32:(t + 1) * 32], lhsT=ones_b, rhs=ohb[:, tp, :], start=(tp == 0), stop=False)
                    return e.matmul(psC[:, t * 32:(t + 1) * 32], lhsT=utri, rhs=ohb[:, t, :], start=(t == 0), stop=True)
                add("pe", fC, reads=["ohb", "cb"], writes=["psC"])
            add("dve", lambda e: e.tensor_tensor(out=tmpc[:], in0=psC[:, :].rearrange("p (t k) -> p t k", t=NT),
                                                 in1=cfs("ebase").unsqueeze(1).to_broadcast([128, NT, 32]), op=ALU.add),
                reads=["psC", "cf"], writes=["tmpc"])
            for ohx, dxf, dxi, nm in ((oh1, d1f, d1i, "d1"), (oh2, d2f, d2i, "d2")):
                add("dve", lambda e, ohx=ohx: e.tensor_tensor(out=tmpd[:], in0=tmpc[:], in1=ohx[:], op=ALU.mult),
                    reads=["tmpc", "oh1", "oh2"], writes=["tmpd"])
                for w_ in (16, 8, 4, 2, 1):
                    add("dve", lambda e, w_=w_: e.tensor_tensor(out=tmpd[:, :, 0:w_], in0=tmpd[:, :, 0:w_], in1=tmpd[:, :, w_:2 * w_], op=ALU.add),
                        reads=["tmpd"], writes=["tmpd"])
                add("dve", lambda e, dxf=dxf: e.tensor_copy(out=dxf[:].unsqueeze(2), in_=tmpd[:, :, 0:1]), reads=["tmpd"], writes=[nm + "f"])
                add("dve", lambda e, dxf=dxf, dxi=dxi: e.tensor_copy(out=dxi[:], in_=dxf[:]), reads=[nm + "f"], writes=[nm + "i"])
            if debug:
                dump("lg", lg[:], [128, NT, 36], F32, LG)
                dump("d1f", d1f[:], [128, NT], F32, ["d1f"])
                dump("d2f", d2f[:], [128, NT], F32, ["d2f"])
                dump("g1", g1[:], [128, NT], F32, ["g1"])
                dump("g2", g2[:], [128, NT], F32, ["g2"])
            for t in range(NT):
                b = t % 2
                add("sp", lambda e, t=t, b=b: e.dma_start(out=xg[b][:], in_=xn2_d[t * 128:(t + 1) * 128, :]), reads=[("xn2_d", t)], writes=[("xg", b)], dma=True)
                for dxi, nm in ((d1i, "d1i"), (d2i, "d2i")):
                    add("pool", lambda e, t=t, b=b, dxi=dxi: e.indirect_dma_start(
                        out=xdisp_d, out_offset=bass.IndirectOffsetOnAxis(ap=dxi[:, t:t + 1], axis=0), in_=xg[b][:], in_offset=None),
                        reads=[("xg", b), nm], writes=["xdisp_d"], dma=True)
            sch.flush()

        if upto >= 7:
          with ExitStack() as st:
            xs = [sb(st, "xs%d" % i, [128, 2, D], BF16) for i in range(2)]
            xT = [sb(st, "xTe%d" % i, [128, 16, CAP], BF16) for i in range(2)]
            hT = sb(st, "hT", [128, 8, CAP], BF16)
            sgl = [sb(st, "sgl%d" % i, [128, CAP], F32) for i in range(2)]
            ys = [sb(st, "ys%d" % i, [128, D], F32) for i in range(4)]
            pTe = [ps(st, "pTe%d" % i, [128, 1024], BF16) for i in range(2)]
            psG = [ps(st, "psG%d" % i, [128, 512], F32) for i in range(4)]
            psY = [ps(st, "psY%d" % i, [128, 512], F32) for i in range(2)]

            NE = int(os.environ.get("MOE_EXPERTS", NEXP))
            evc = [0]
            gcnt = [0]
            ycnt = [0]
            def xs_load(ex):
                xb = ex % 2
                add("sp", lambda e: e.dma_start(out=xs[xb][:], in_=xdisp_d[ex * CAP:(ex + 1) * CAP, :].rearrange("(s p) d -> p s d", p=128)),
                    reads=["xdisp_d"], writes=[("xs", xb)], dma=True)

            xs_load(0)
            for ex in range(NE):
                xb = ex % 2
                units = {}
                units["g0"] = get_unit(ex, "g0", w_eg[ex], False, 0)
                units["u0"] = get_unit(ex, "u0", w_eu[ex], False, 0)
                for s_ in range(2):
                    for hh in range(2):
                        pt = evc[0] % 2
                        evc[0] += 1

                        def trx(e, s_=s_, hh=hh, pt=pt, xb=xb):
                            ins = None
                            for c8 in range(8):
                                c = hh * 8 + c8
                                ins = e.transpose(out=pTe[pt][:, c8 * 128:(c8 + 1) * 128], in_=xs[xb][:, s_, c * 128:(c + 1) * 128], identity=ident)
                            return ins
                        add("pe", trx, reads=[("xs", xb), "cb"], writes=[("pTe", pt)])
                        dstx = xT[xb][:, hh * 8:(hh + 1) * 8, s_ * 128:(s_ + 1) * 128]
                        srcx = pTe[pt][:].rearrange("p (c n) -> p c n", c=8)
                        if pt == 0:
                            add("act", lambda e, dstx=dstx, srcx=srcx: e.activation(out=dstx, in_=srcx, func=AF.Copy),
                                reads=[("pTe", pt)], writes=[("xT", xb, s_, hh)])
                        else:
                            add("dve", lambda e, dstx=dstx, srcx=srcx: e.tensor_copy(out=dstx, in_=srcx),
                                reads=[("pTe", pt)], writes=[("xT", xb, s_, hh)])
                XTK = [("xT", xb, a, c) for a in range(2) for c in range(2)]
                if ex + 1 < NE:
                    xs_load(ex + 1)
                for hf in range(2):
                    if hf == 1:
                        units["g1"] = get_unit(ex, "g1", w_eg[ex], False, 1)
                        units["u1"] = get_unit(ex, "u1", w_eu[ex], False, 1)
                    ug = units["g%d" % hf]
                    uu = units["u%d" % hf]
                    for fcl in range(4):
                        fc = hf * 4 + fcl
                        pg = (gcnt[0] % 2) * 2
                        gcnt[0] += 1
                        for which, un in ((0, ug), (1, uu)):
                            def fG(e, which=which, un=un, fcl=fcl, pg=pg, xb=xb):
                                ins = None
                                wv = wu[un][:].rearrange("p (c f) -> p c f", c=16)
                                for c in range(16):
                                    ins = e.matmul(psG[pg + which][:, 0:CAP], lhsT=wv[:, c, fcl * 128:(fcl + 1) * 128], rhs=xT[xb][:, c, :],
                                                   start=(c == 0), stop=(c == 15))
                                return ins
                            add("pe", fG, reads=[("wu", un)] + XTK, writes=[("psG", pg + which)])
                        q = fc % 2
                        add("act", lambda e, pg=pg, q=q: e.activation(out=sgl[q][:], in_=psG[pg][:, 0:CAP], func=AF.Silu),
                            reads=[("psG", pg)], writes=[("sgl", q)])
                        add("dve", lambda e, pg=pg, q=q, fc=fc: e.tensor_tensor(out=hT[:, fc, :], in0=psG[pg + 1][:, 0:CAP], in1=sgl[q][:], op=ALU.mult),
                            reads=[("psG", pg + 1), ("sgl", q)], writes=[("hT", fc)])
                HTK = [("hT", fc) for fc in range(8)]
                yb = [(ycnt[0] * 2 + s_) % 4 for s_ in range(2)]
                ycnt[0] += 1
                for dh in range(2):
                    ud = get_unit(ex, "d%d" % dh, w_ed[ex], True, dh)
                    for s_ in range(2):
                        for nb in range(2):
                            py = (s_ * 2 + nb) % 2

                            def fY(e, ud=ud, s_=s_, nb=nb, py=py):
                                ins = None
                                wv = wu[ud][:].rearrange("p (c f) -> p c f", c=8)
                                for fc in range(8):
                                    ins = e.matmul(psY[py][:, :], lhsT=hT[:, fc, s_ * 128:(s_ + 1) * 128], rhs=wv[:, fc, nb * 512:(nb + 1) * 512],
                                                   start=(fc == 0), stop=(fc == 7))
                                return ins
                            add("pe", fY, reads=[("wu", ud)] + HTK, writes=[("psY", py)])
                            col = dh * 1024 + nb * 512
                            ydst = ys[yb[s_]][:, col:col + 512]
                            if nb == 0:
                                add("act", lambda e, ydst=ydst, py=py: e.activation(out=ydst, in_=psY[py][:, :], func=AF.Copy),
                                    reads=[("psY", py)], writes=[("ys", yb[s_], dh, nb)])
                            else:
                                add("dve", lambda e, ydst=ydst, py=py: e.tensor_copy(out=ydst, in_=psY[py][:, :]),
                                    reads=[("psY", py)], writes=[("ys", yb[s_], dh, nb)])
                for s_ in range(2):
                    add("sp", lambda e, ex=ex, s_=s_, ybs=yb[s_]: e.dma_start(out=ydisp_d[ex * CAP + s_ * 128:ex * CAP + (s_ + 1) * 128, :], in_=ys[ybs][:]),
                        reads=[("ys", yb[s_], dh, nb) for dh in range(2) for nb in range(2)], writes=["ydisp_d"], dma=True)
            sch.flush()

        GW.close()
        if upto >= 8:
          with ExitStack() as st:
            y1 = [sb(st, "y1_%d" % i, [128, D], F32) for i in range(3)]
            y2 = [sb(st, "y2_%d" % i, [128, D], F32) for i in range(3)]
            hh1 = [sb(st, "hh1_%d" % i, [128, D], F32) for i in range(3)]
            ot = [sb(st, "ot%d" % i, [128, D], F32) for i in range(3)]
            junk4 = sb(st, "junk4", [128, D], BF16)
            gfin = sb(st, "gfin", [128, D], F32)
            ss3 = sb(st, "ss3", [128, NT], F32)
            rs3 = sb(st, "rs3", [128, NT], F32)
            add("sp", lambda e: e.dma_start(out=gfin[:], in_=g_fin[0:1, :].to_broadcast([128, D])), writes=["gfin"], dma=True)
            def h_load(t):
                b = t % 3
                rows = slice(t * 128, (t + 1) * 128)
                add("sp", lambda e: e.dma_start(out=hh1[b][:], in_=h1_d[rows, :]), reads=[("h1_d", t)], writes=[("hh1", b)], dma=True)

            h_load(0)
            h_load(1)
            for t in range(NT):
                b = t % 3
                rows = slice(t * 128, (t + 1) * 128)
                add("pool", lambda e, t=t, b=b: e.indirect_dma_start(
                    out=y1[b][:], out_offset=None, in_=ydisp_d, in_offset=bass.IndirectOffsetOnAxis(ap=d1i[:, t:t + 1], axis=0)),
                    reads=["ydisp_d", "d1i"], writes=[("y1", b)], dma=True)
                add("pool", lambda e, t=t, b=b: e.indirect_dma_start(
                    out=y2[b][:], out_offset=None, in_=ydisp_d, in_offset=bass.IndirectOffsetOnAxis(ap=d2i[:, t:t + 1], axis=0)),
                    reads=["ydisp_d", "d2i"], writes=[("y2", b)], dma=True)
                add("dve", lambda e, t=t, b=b: e.scalar_tensor_tensor(out=hh1[b][:], in0=y1[b][:], scalar=g1[:, t:t + 1], in1=hh1[b][:],
                                                                      op0=ALU.mult, op1=ALU.add),
                    reads=[("y1", b), ("hh1", b), "g1"], writes=[("hh1", b)])
                add("dve", lambda e, t=t, b=b: e.scalar_tensor_tensor(out=hh1[b][:], in0=y2[b][:], scalar=g2[:, t:t + 1], in1=hh1[b][:],
                                                                      op0=ALU.mult, op1=ALU.add),
                    reads=[("y2", b), ("hh1", b), "g2"], writes=[("hh1", b)])
                add("act", lambda e, t=t, b=b: e.activation(out=junk4[:], in_=hh1[b][:], func=AF.Square, accum_out=ss3[:, t:t + 1]),
                    reads=[("hh1", b)], writes=["junk4", ("ss3", t)])
                add("act", lambda e, t=t: e.activation(out=rs3[:, t:t + 1], in_=ss3[:, t:t + 1], func=AF.Sqrt, scale=1.0 / D, bias=EPS),
                    reads=[("ss3", t)], writes=[("rs3", t)])
                add("dve", lambda e, t=t: e.reciprocal(out=rs3[:, t:t + 1], in_=rs3[:, t:t + 1]), reads=[("rs3", t)], writes=[("rs3", t)])
                add("dve", lambda e, t=t, b=b: e.scalar_tensor_tensor(out=ot[b][:], in0=hh1[b][:], scalar=rs3[:, t:t + 1], in1=gfin[:],
                                                                      op0=ALU.mult, op1=ALU.mult),
                    reads=[("hh1", b), ("rs3", t), "gfin"], writes=[("ot", b)])
                if t + 2 < NT:
                    h_load(t + 2)
                add("sp", lambda e, b=b, rows=rows: e.dma_start(out=out[rows, :], in_=ot[b][:]), reads=[("ot", b)], writes=[("out", t)], dma=True)
            sch.flush()
    return nc


_NC_CACHE = {}


def kernel(**inputs):
    x = np.asarray(inputs["x"], np.float32)
    B = x.shape[0]
    if "nc" not in _NC_CACHE:
        _NC_CACHE["nc"] = build()
    nc = _NC_CACHE["nc"]
    cbv, cfv, _ = make_consts()
    shared = {"cb": cbv, "cf": cfv}
    for k, v in inputs.items():
        if k == "x":
            continue
        a = np.asarray(v, np.float32)
        if k == "g_final":
            a = a.reshape(1, -1)
        else:
            a = a[0]
            if a.ndim == 1:
                a = a.reshape(1, -1)
        shared[k] = np.ascontiguousarray(a)
    in_maps = []
    for b in range(B):
        m = dict(shared)
        m["x"] = np.ascontiguousarray(x[b])
        in_maps.append(m)
    res = run_bass_kernel_spmd(nc, in_maps, core_ids=list(range(B)))
    return np.stack([np.asarray(r["out"], np.float32) for r in res.results], axis=0)
```

```python
import os
from contextlib import ExitStack
import numpy as np
import ml_dtypes
import concourse.bass as bass
import concourse.mybir as mybir
from concourse.bass_utils import run_bass_kernel_spmd

F32 = mybir.dt.float32
BF16 = mybir.dt.bfloat16
I32 = mybir.dt.int32
U32 = mybir.dt.uint32
ALU = mybir.AluOpType
AF = mybir.ActivationFunctionType
AX = mybir.AxisListType

S = 2048
D = 2048
NT = 16
EPS = 1e-6
D_IN = 9928
C_QLAT, C_CKV, C_QIDX, C_KIDX, C_WIDX, C_QR, C_KR, C_VR, C_GR, C_GBR = (
    0, 1024, 1152, 1664, 1728, 1736, 2760, 3784, 4808, 5832)
NEXP = 32
CAP = 256
FEXP = 1024
NEG = -1.0e30
MBIG = 30000.0


class _Op:
    __slots__ = ("eng", "fn", "dma", "deps", "signal", "token", "idx")


class Sched:
    ENGS = ("pe", "dve", "act", "pool", "sp")

    def __init__(self, nc, stack, ndma=8):
        self.nc = nc
        self.ops = []
        self.lastw = {}
        self.readers = {}
        self.ndma = ndma
        self.csem = {e: stack.enter_context(nc.semaphore("cs_" + e)) for e in ("pe", "dve", "act", "pool")}
        self.dsem = {e: [stack.enter_context(nc.semaphore("ds_%s%d" % (e, i))) for i in range(ndma)]
                     for e in ("sp", "act", "pool")}
        self.ccount = {e: 0 for e in self.csem}
        self.qcount = {e: 0 for e in self.dsem}
        self.suses = {e: [0] * ndma for e in self.dsem}
        self.sprev = {e: [None] * ndma for e in self.dsem}
        self.waited = {e: {} for e in self.ENGS}
        self.flushed = 0

    def add(self, eng, fn, reads=(), writes=(), dma=False):
        op = _Op()
        op.eng, op.fn, op.dma = eng, fn, dma
        op.deps = set()
        op.signal = dma
        op.token = None
        op.idx = len(self.ops)

        def dep(p, raw):
            if p is op:
                return
            if (not p.dma) and p.eng == eng and (not dma):
                if eng == "pe":
                    return
            op.deps.add(p)

        for k in reads:
            p = self.lastw.get(k)
            if p is not None:
                dep(p, True)
        for k in writes:
            p = self.lastw.get(k)
            if p is not None:
                dep(p, False)
            for r in self.readers.get(k, ()):
                dep(r, False)
        for k in reads:
            self.readers.setdefault(k, []).append(op)
        for k in writes:
            self.lastw[k] = op
            self.readers[k] = []
        self.ops.append(op)
        return op

    def flush(self):
        nc = self.nc
        ops = self.ops[self.flushed:]
        for op in ops:
            if op.dma:
                e = op.eng
                slot = self.qcount[e] % self.ndma
                self.qcount[e] += 1
                prev = self.sprev[e][slot]
                if prev is not None:
                    op.deps.add(prev)
                self.suses[e][slot] += 1
                op.token = (self.dsem[e][slot], 16 * self.suses[e][slot])
                self.sprev[e][slot] = op
        for op in ops:
            for d in op.deps:
                d.signal = True
        for op in ops:
            if op.signal and not op.dma and op.token is None:
                self.ccount[op.eng] += 1
                op.token = (self.csem[op.eng], self.ccount[op.eng])
        fence = _Op()
        fence.eng, fence.fn, fence.dma, fence.signal, fence.token = "sp", None, False, False, None
        fence.deps = set(op for op in ops if op.dma)
        fence.idx = -1
        by_eng = {e: [] for e in self.ENGS}
        for op in ops:
            by_eng[op.eng].append(op)
        by_eng["sp"].append(fence)
        self.flushed = len(self.ops)

        def emit(ename, e):
            waited = self.waited[ename]
            for op in by_eng[ename]:
                need = {}
                for d in op.deps:
                    if d.token is None:
                        continue
                    sem, val = d.token
                    key = id(sem)
                    if waited.get(key, 0) < val and need.get(key, (None, 0))[1] < val:
                        need[key] = (sem, val)
                for key, (sem, val) in need.items():
                    e.wait_ge(sem, val)
                    waited[key] = val
                if op.fn is None:
                    continue
                ins = op.fn(e)
                if op.signal:
                    sem, _ = op.token
                    ins.then_inc(sem, 16 if op.dma else 1)

        with nc.Block() as block:
            if by_eng["pe"]:
                @block.tensor
                def _(e):
                    emit("pe", e)
            if by_eng["dve"]:
                @block.vector
                def _(e):
                    emit("dve", e)
            if by_eng["act"]:
                @block.scalar
                def _(e):
                    emit("act", e)
            if by_eng["pool"]:
                @block.gpsimd
                def _(e):
                    emit("pool", e)

            @block.sync
            def _(e):
                emit("sp", e)


CB = {}
CF = {}


def _layout():
    off = 0
    for name, w in (("ident", 128), ("ident8", 1024), ("ones", 128), ("utri", 128)):
        CB[name] = (off, w)
        off += w
    CB["_n"] = off
    off = 0
    for name, w in (("cos", NT * 64), ("sin", NT * 64), ("dq", 8 * 128), ("gk", 8), ("kdec", 8),
                    ("cmask", 128), ("ebase", NEXP), ("iota_e", NEXP)):
        CF[name] = (off, w)
        off += w
    CF["_n"] = off


_layout()


def make_consts():
    cb = np.zeros((128, CB["_n"]), np.float32)
    eye = np.eye(128, dtype=np.float32)
    o, w = CB["ident"]; cb[:, o:o + w] = eye
    o, w = CB["ident8"]; cb[:, o:o + w] = np.tile(eye, (1, 8))
    o, w = CB["ones"]; cb[:, o:o + w] = 1.0
    o, w = CB["utri"]; cb[:, o:o + w] = np.triu(np.ones((128, 128), np.float32), 1)
    cf = np.zeros((128, CF["_n"]), np.float32)
    half = 64
    freq = (10000.0 ** (-np.arange(half, dtype=np.float32) / half)).astype(np.float32)
    pos = np.arange(S, dtype=np.float32)
    ang = (pos[:, None] * freq[None, :]).astype(np.float32)
    cos = np.cos(ang).astype(np.float32).reshape(NT, 128, 64).transpose(1, 0, 2).reshape(128, NT * 64)
    sin = np.sin(ang).astype(np.float32).reshape(NT, 128, 64).transpose(1, 0, 2).reshape(128, NT * 64)
    o, w = CF["cos"]; cf[:, o:o + w] = cos
    o, w = CF["sin"]; cf[:, o:o + w] = sin
    lg = np.log1p(-np.exp2(-5.0 - np.arange(8, dtype=np.float64)))
    n = np.arange(128, dtype=np.float64)
    dq = np.exp(lg[:, None] * (n[None, :] + 1.0)) * (128.0 ** -0.5)
    o, w = CF["dq"]; cf[:, o:o + w] = dq.reshape(1, 1024)
    o, w = CF["gk"]; cf[:, o:o + w] = np.exp(-lg[None, :] * (n[:, None] + 1.0))
    o, w = CF["kdec"]; cf[:, o:o + w] = np.exp(lg[None, :] * (127.0 - n[:, None]))
    o, w = CF["cmask"]; cf[:, o:o + w] = np.triu(np.ones((128, 128)), 0)
    o, w = CF["ebase"]; cf[:, o:o + w] = (np.arange(NEXP) * CAP)[None, :]
    o, w = CF["iota_e"]; cf[:, o:o + w] = np.arange(NEXP)[None, :]
    g128 = [float(np.exp(lg[h] * 128.0)) for h in range(8)]
    return cb.astype(ml_dtypes.bfloat16), cf.astype(np.float32), g128


_G128 = make_consts()[2]


def build(debug=False, upto=99):
    nc = bass.Bass("TRN2", target_bir_lowering=False)
    dk = "ExternalOutput" if debug else "Internal"

    def din(name, shape, dt=F32):
        return nc.dram_tensor(name, list(shape), dt, kind="ExternalInput").ap()

    def dscr(name, shape, dt):
        return nc.dram_tensor(name, list(shape), dt, kind=dk).ap()

    x = din("x", [S, D])
    g_mix = din("g_mix_norm", [1, D])
    w_in = din("w_in", [D, D_IN])
    g_kv = din("g_kv", [1, 128])
    w_uv = din("w_uv", [8, 128, 128])
    g_ret = din("g_ret", [1, 1024])
    w_branch = din("w_branch", [2, 1024, D])
    w_out = din("w_out", [D, D])
    g_ffn = din("g_ffn_norm", [1, D])
    w_rg = din("w_router_group", [D, 4])
    b_rg = din("b_router_group", [1, 4])
    w_re = din("w_router_expert", [D, NEXP])
    b_re = din("b_router_expert", [1, NEXP])
    w_eg = din("w_expert_gate", [NEXP, D, FEXP])
    w_eu = din("w_expert_up", [NEXP, D, FEXP])
    w_ed = din("w_expert_down", [NEXP, FEXP, D])
    g_fin = din("g_final", [1, D])
    cbd = din("cb", [128, CB["_n"]], BF16)
    cfd = din("cf", [128, CF["_n"]], F32)
    out = nc.dram_tensor("out", [S, D], F32, kind="ExternalOutput").ap()

    qlatT_d = dscr("qlatT_d", [1024, S], BF16)
    qidxT_d = dscr("qidxT_d", [512, S], BF16)
    qrot_d = dscr("qrot_d", [S, 1024], BF16)
    krot_d = dscr("krot_d", [S, 1024], BF16)
    kdec_d = dscr("kdec_d", [S, 1024], BF16)
    vr_d = dscr("vr_d", [S, 1024], BF16)
    gr_d = dscr("gr_d", [S, 1024], BF16)
    gbT_d = dscr("gbT_d", [4096, S], BF16)
    oaT_d = dscr("oaT_d", [1024, S], BF16)
    obT_d = dscr("obT_d", [1024, S], BF16)
    h1_d = dscr("h1_d", [S, D], F32)
    xn2_d = dscr("xn2_d", [S, D], BF16)
    xdisp_d = dscr("xdisp_d", [NEXP * CAP, D], BF16)
    ydisp_d = dscr("ydisp_d", [NEXP * CAP, D], F32)
    dbg_d = dscr("dbg_d", [128, 4096], F32)
    NF32 = int(os.environ.get("MOE_F32_EXPERTS", 16))
    NCV = NEXP - NF32
    wgb_d = nc.dram_tensor("wgb_d", [NCV, D, FEXP], BF16).ap()
    wub_d = nc.dram_tensor("wub_d", [NCV, D, FEXP], BF16).ap()
    wdb_d = nc.dram_tensor("wdb_d", [NCV, FEXP, D], BF16).ap()

    with ExitStack() as P:
        sch = Sched(nc, P)
        add = sch.add

        def sb(st, name, shape, dt):
            return st.enter_context(nc.sbuf_tensor(name, list(shape), dt))

        def ps(st, name, shape, dt):
            return st.enter_context(nc.psum_tensor(name, list(shape), dt))

        def conv_gen():
            for ex in range(NF32, NEXP):
                for kind, src, dst in (("g", w_eg, wgb_d), ("u", w_eu, wub_d), ("d", w_ed, wdb_d)):
                    add("pool", lambda e, ex=ex, src=src, dst=dst: e.dma_start(
                        out=dst[ex - NF32].rearrange("(c p) n -> p c n", p=128), in_=src[ex].rearrange("(c p) n -> p c n", p=128)),
                        writes=[("wconv", ex, kind)], dma=True)
                    yield
        conv_it = conv_gen()

        def conv(n):
            if upto < 7:
                return
            for _ in range(n):
                try:
                    next(conv_it)
                except StopIteration:
                    return

        cb = sb(P, "cb_sb", [128, CB["_n"]], BF16)
        cf = sb(P, "cf_sb", [128, CF["_n"]], F32)
        add("sp", lambda e: e.dma_start(out=cb[:], in_=cbd), writes=["cb"], dma=True)
        add("sp", lambda e: e.dma_start(out=cf[:], in_=cfd), writes=["cf"], dma=True)

        def cbs(name):
            o, w = CB[name]
            return cb[:, o:o + w]

        def cfs(name):
            o, w = CF[name]
            return cf[:, o:o + w]

        ident = cbs("ident")

        def dump(name, src, shape, dt, reads):
            dd = nc.dram_tensor("dbg_" + name, list(shape), dt, kind="ExternalOutput").ap()
            add("sp", lambda e: e.dma_start(out=dd, in_=src), reads=reads, writes=["dbg_" + name], dma=True)

        KC = ExitStack()
        kvT = sb(KC, "kvT", [128, S], BF16)
        kvtok = sb(KC, "kvtok", [128, NT, 128], BF16)
        kidxT = sb(KC, "kidxT", [128, S], BF16)
        wabs = sb(KC, "wabs", [128, NT, 8], F32)
        wsgn = sb(KC, "wsgn", [128, NT, 8], F32)

        with ExitStack() as st:
            xnT = sb(st, "xnT", [128, NT, S], BF16)
            gbc = sb(st, "gbc", [128, D], F32)
            gkvbc = sb(st, "gkvbc", [128, 128], F32)
            ss = sb(st, "ss", [128, NT], F32)
            rstd = sb(st, "rstd", [128, NT], F32)
            xt = [sb(st, "xt%d" % i, [128, D], F32) for i in range(2)]
            xnb = [sb(st, "xnb%d" % i, [128, D], BF16) for i in range(2)]
            junk = sb(st, "junk", [128, D], BF16)
            pT = [ps(st, "pT%d" % i, [128, 1024], BF16) for i in range(2)]
            pG = [ps(st, "pG%d" % i, [128, 512], F32) for i in range(4)]
            pM = [ps(st, "pM%d" % i, [128, 512], F32) for i in range(2)]

            add("sp", lambda e: e.dma_start(out=gbc[:], in_=g_mix[0:1, :].to_broadcast([128, D])), writes=["gbc"], dma=True)
            zt = sb(st, "zt", [128, D], BF16)
            add("pool", lambda e: e.memset(zt[:], 0.0), writes=["zt"])
            for zi in range(4):
                add("sp", lambda e, zi=zi: e.dma_start(
                    out=xdisp_d[zi * 2048:(zi + 1) * 2048, :].rearrange("(r p) d -> p r d", p=128),
                    in_=zt[:].unsqueeze(1).to_broadcast([128, 16, D])),
                    reads=["zt"], writes=["xdisp_d"], dma=True)
            add("sp", lambda e: e.dma_start(out=gkvbc[:], in_=g_kv[0:1, :].to_broadcast([128, 128])), writes=["gkvbc"], dma=True)
            for t in range(NT):
                b = t % 2
                add("sp", lambda e, t=t, b=b: e.dma_start(out=xt[b][:], in_=x[t * 128:(t + 1) * 128, :]),
                    writes=[("xt", b)], dma=True)
                add("act", lambda e, t=t, b=b: e.activation(out=junk[:], in_=xt[b][:], func=AF.Square,
                                                            accum_out=ss[:, t:t + 1]),
                    reads=[("xt", b)], writes=["junk", ("ss", t)])
                add("act", lambda e, t=t: e.activation(out=rstd[:, t:t + 1], in_=ss[:, t:t + 1], func=AF.Sqrt,
                                                       scale=1.0 / D, bias=EPS),
                    reads=[("ss", t)], writes=[("rstd", t)])
                add("dve", lambda e, t=t: e.reciprocal(out=rstd[:, t:t + 1], in_=rstd[:, t:t + 1]),
                    reads=[("rstd", t)], writes=[("rstd", t)])
                add("dve", lambda e, t=t, b=b: e.scalar_tensor_tensor(out=xnb[b][:], in0=xt[b][:], scalar=rstd[:, t:t + 1],
                                                                      in1=gbc[:], op0=ALU.mult, op1=ALU.mult),
                    reads=[("xt", b), ("rstd", t), "gbc"], writes=[("xnb", b)])

                def tr(e, t=t, b=b):
                    ins = None
                    for c in range(16):
                        ins = e.transpose(out=pT[c // 8][:, (c % 8) * 128:(c % 8 + 1) * 128],
                                          in_=xnb[b][:, c * 128:(c + 1) * 128], identity=ident)
                    return ins
                add("pe", tr, reads=[("xnb", b), "cb"], writes=["pT0", "pT1"])
                add("act", lambda e, t=t: e.activation(out=xnT[:, 0:8, t * 128:(t + 1) * 128],
                                                       in_=pT[0][:].rearrange("p (c n) -> p c n", c=8), func=AF.Copy),
                    reads=["pT0"], writes=[("xnT", t)])
                add("dve", lambda e, t=t: e.tensor_copy(out=xnT[:, 8:16, t * 128:(t + 1) * 128],
                                                        in_=pT[1][:].rearrange("p (c n) -> p c n", c=8)),
                    reads=["pT1"], writes=[("xnT", t)])
            XN_ALL = [("xnT", t) for t in range(NT)]

            NW = 3
            wr = [sb(st, "wr%d" % i, [128, 16, 512], BF16) for i in range(NW)]
            stg = [sb(st, "stg%d" % i, [128, 2048], BF16) for i in range(2)]
            stg2 = [sb(st, "stgb%d" % i, [128, 2048], BF16) for i in range(2)]
            tmp = [sb(st, "rtmp%d" % i, [128, 4, 64], F32) for i in range(4)]
            wcnt = [0]
            pcnt = [0]
            scnt = [0]
            ecnt = [0]

            def load_w(c0, width, dup=False):
                slot = wcnt[0] % NW
                wcnt[0] += 1
                src = w_in[:, c0:c0 + width].rearrange("(c p) n -> p c n", p=128)
                if dup:
                    add("pool", lambda e: e.dma_start(out=wr[slot][:, :, 0:width], in_=src), writes=[("wr", slot)], dma=True)
                    add("pool", lambda e: e.dma_start(out=wr[slot][:, :, width:2 * width], in_=src), writes=[("wr", slot)], dma=True)
                else:
                    add("pool", lambda e: e.dma_start(out=wr[slot][:, :, 0:width], in_=src), writes=[("wr", slot)], dma=True)
                if wcnt[0] > 3:
                    conv(1)
                return slot

            def gemm_T(slot, t, width):
                pb = pcnt[0] % 4
                pcnt[0] += 1

                def f(e):
                    ins = None
                    for c in range(16):
                        ins = e.matmul(pG[pb][:, 0:width], lhsT=xnT[:, c, t * 128:(t + 1) * 128], rhs=wr[slot][:, c, 0:width],
                                       start=(c == 0), stop=(c == 15))
                    return ins
                add("pe", f, reads=[("wr", slot), ("xnT", t)], writes=[("pG", pb)])
                return pb

            def gemm_F(slot, j, tb):
                pb = pcnt[0] % 4
                pcnt[0] += 1

                def f(e):
                    ins = None
                    for c in range(16):
                        ins = e.matmul(pG[pb][:, :], lhsT=wr[slot][:, c, j * 128:(j + 1) * 128], rhs=xnT[:, c, tb * 512:(tb + 1) * 512],
                                       start=(c == 0), stop=(c == 15))
                    return ins
                add("pe", f, reads=[("wr", slot)] + [("xnT", tb * 4 + i) for i in range(4)], writes=[("pG", pb)])
                return pb

            def evac_copy(pb, dst, dkeys, width=512, func=None):
                if func is not None:
                    add("act", lambda e: e.activation(out=dst, in_=pG[pb][:, 0:width], func=func),
                        reads=[("pG", pb)], writes=dkeys)
                    return
                ecnt[0] += 1
                if ecnt[0] % 2 == 0:
                    add("act", lambda e: e.activation(out=dst, in_=pG[pb][:, 0:width], func=AF.Copy),
                        reads=[("pG", pb)], writes=dkeys)
                else:
                    add("dve", lambda e: e.tensor_copy(out=dst, in_=pG[pb][:, 0:width]),
                        reads=[("pG", pb)], writes=dkeys)

            def block_F(c0, dst_d, row0, func=None):
                slot = load_w(c0, 512)
                for j in range(4):
                    sslot = scnt[0] % 2
                    scnt[0] += 1
                    for tb in range(4):
                        pb = gemm_F(slot, j, tb)
                        evac_copy(pb, stg[sslot][:, tb * 512:(tb + 1) * 512], [("stg", sslot, tb)], func=func)
                    add("sp", lambda e, j=j, sslot=sslot: e.dma_start(out=dst_d[row0 + j * 128:row0 + (j + 1) * 128, :], in_=stg[sslot][:]),
                        reads=[("stg", sslot, tb) for tb in range(4)], writes=[(id(dst_d), row0, j)], dma=True)

            def block_T(c0, dst_d, col0, kind):
                slot = load_w(c0, 512)
                hb = (col0 // 512) * 4
                for t4 in range(4):
                    sslot = scnt[0] % 2
                    scnt[0] += 1
                    for i in range(4):
                        t = t4 * 4 + i
                        pb = gemm_T(slot, t, 512)
                        dst = stg[sslot][:, i * 512:(i + 1) * 512]
                        dk_ = [("stg", sslot, i)]
                        if kind == "copy":
                            evac_copy(pb, dst, dk_)
                        elif kind == "silu":
                            evac_copy(pb, dst, dk_, func=AF.Silu)
                        else:
                            pv = pG[pb][:].rearrange("p (h two d) -> p h two d", h=4, two=2)
                            dv = dst.rearrange("p (h two d) -> p h two d", h=4, two=2)
                            cosb = cfs("cos")[:, t * 64:(t + 1) * 64].unsqueeze(1).to_broadcast([128, 4, 64])
                            sinb = cfs("sin")[:, t * 64:(t + 1) * 64].unsqueeze(1).to_broadcast([128, 4, 64])
                            rk = [("rtmp", q) for q in range(4)]
                            add("dve", lambda e, pv=pv, cosb=cosb: e.tensor_tensor(out=tmp[0][:], in0=pv[:, :, 0, :], in1=cosb, op=ALU.mult),
                                reads=[("pG", pb), "cf"], writes=[rk[0]])
                            add("dve", lambda e, pv=pv, sinb=sinb: e.tensor_tensor(out=tmp[1][:], in0=pv[:, :, 1, :], in1=sinb, op=ALU.mult),
                                reads=[("pG", pb), "cf"], writes=[rk[1]])
                            add("dve", lambda e, pv=pv, sinb=sinb: e.tensor_tensor(out=tmp[2][:], in0=pv[:, :, 0, :], in1=sinb, op=ALU.mult),
                                reads=[("pG", pb), "cf"], writes=[rk[2]])
                            add("dve", lambda e, pv=pv, cosb=cosb: e.tensor_tensor(out=tmp[3][:], in0=pv[:, :, 1, :], in1=cosb, op=ALU.mult),
                                reads=[("pG", pb), "cf"], writes=[rk[3]])
                            add("dve", lambda e, dv=dv: e.tensor_tensor(out=dv[:, :, 0, :], in0=tmp[0][:], in1=tmp[1][:], op=ALU.subtract),
                                reads=[rk[0], rk[1]], writes=dk_)
                            add("dve", lambda e, dv=dv: e.tensor_tensor(out=dv[:, :, 1, :], in0=tmp[2][:], in1=tmp[3][:], op=ALU.add),
                                reads=[rk[2], rk[3]], writes=dk_)
                            if kind == "rotk":
                                d2 = stg2[sslot][:, i * 512:(i + 1) * 512].rearrange("p (h d) -> p h d", h=4)
                                kd = cfs("kdec")[:, hb:hb + 4].unsqueeze(2).to_broadcast([128, 4, 128])
                                add("pool", lambda e, d2=d2, kd=kd, dst=dst: e.tensor_tensor(
                                    out=d2, in0=dst.rearrange("p (h d) -> p h d", h=4), in1=kd, op=ALU.mult),
                                    reads=dk_ + ["cf"], writes=[("stg2", sslot, i)])
                    dview = dst_d[t4 * 512:(t4 + 1) * 512, col0:col0 + 512].rearrange("(i p) n -> p i n", p=128)
                    add("sp", lambda e, dview=dview, sslot=sslot: e.dma_start(out=dview, in_=stg[sslot][:].rearrange("p (i n) -> p i n", i=4)),
                        reads=[("stg", sslot, i) for i in range(4)], writes=[(id(dst_d), col0, t4)], dma=True)
                    if kind == "rotk":
                        dview2 = kdec_d[t4 * 512:(t4 + 1) * 512, col0:col0 + 512].rearrange("(i p) n -> p i n", p=128)
                        add("sp", lambda e, dview2=dview2, sslot=sslot: e.dma_start(out=dview2, in_=stg2[sslot][:].rearrange("p (i n) -> p i n", i=4)),
                            reads=[("stg2", sslot, i) for i in range(4)], writes=[("kdec_d", col0, t4)], dma=True)

            slot = load_w(C_CKV, 128)
            ssk = sb(st, "ssk", [128, NT], F32)
            rsk = sb(st, "rsk", [128, NT], F32)
            junk2 = sb(st, "junk2", [128, 128], F32)
            for t in range(NT):
                pb = gemm_T(slot, t, 128)
                add("act", lambda e, t=t, pb=pb: e.activation(out=junk2[:], in_=pG[pb][:, 0:128], func=AF.Square, accum_out=ssk[:, t:t + 1]),
                    reads=[("pG", pb)], writes=["junk2", ("ssk", t)])
                add("act", lambda e, t=t: e.activation(out=rsk[:, t:t + 1], in_=ssk[:, t:t + 1], func=AF.Sqrt, scale=1.0 / 128, bias=EPS),
                    reads=[("ssk", t)], writes=[("rsk", t)])
                add("dve", lambda e, t=t: e.reciprocal(out=rsk[:, t:t + 1], in_=rsk[:, t:t + 1]),
                    reads=[("rsk", t)], writes=[("rsk", t)])
                add("dve", lambda e, t=t, pb=pb: e.scalar_tensor_tensor(out=kvtok[:, t, :], in0=pG[pb][:, 0:128], scalar=rsk[:, t:t + 1],
                                                                        in1=gkvbc[:], op0=ALU.mult, op1=ALU.mult),
                    reads=[("pG", pb), ("rsk", t), "gkvbc"], writes=[("kvtok", t)])
                add("pe", lambda e, t=t: e.transpose(out=pT[0][:, 0:128], in_=kvtok[:, t, :], identity=ident),
                    reads=[("kvtok", t), "cb"], writes=["pT0"])
                add("act", lambda e, t=t: e.activation(out=kvT[:, t * 128:(t + 1) * 128], in_=pT[0][:, 0:128], func=AF.Copy),
                    reads=["pT0"], writes=[("kvT", t)])
            slot = load_w(C_KIDX, 64, dup=True)
            for tb in range(4):
                pb = gemm_F(slot, 0, tb)
                evac_copy(pb, kidxT[:, tb * 512:(tb + 1) * 512], [("kidxT", tb)])
            slot = load_w(C_WIDX, 8)
            for t in range(NT):
                pb = gemm_T(slot, t, 8)
                add("act", lambda e, t=t, pb=pb: e.activation(out=wabs[:, t, :], in_=pG[pb][:, 0:8], func=AF.Abs),
                    reads=[("pG", pb)], writes=[("wabs", t)])
                add("dve", lambda e, t=t, pb=pb: e.tensor_scalar(out=wsgn[:, t, :], in0=pG[pb][:, 0:8], scalar1=0.0, scalar2=2.0,
                                                                 op0=ALU.is_ge, op1=ALU.mult),
                    reads=[("pG", pb)], writes=[("wsgn", t)])
                add("dve", lambda e, t=t: e.tensor_scalar(out=wsgn[:, t, :], in0=wsgn[:, t, :], scalar1=-1.0, scalar2=None, op0=ALU.add),
                    reads=[("wsgn", t)], writes=[("wsgn", t)])

            if upto >= 2:
                for i in range(2):
                    block_F(C_QLAT + i * 512, qlatT_d, i * 512)
                block_F(C_QIDX, qidxT_d, 0)
                for i in range(2):
                    block_T(C_QR + i * 512, qrot_d, i * 512, "rotq")
                for i in range(2):
                    block_T(C_KR + i * 512, krot_d, i * 512, "rotk")
                for i in range(2):
                    block_T(C_VR + i * 512, vr_d, i * 512, "copy")
                for i in range(2):
                    block_T(C_GR + i * 512, gr_d, i * 512, "silu")
                for i in range(8):
                    block_F(C_GBR + i * 512, gbT_d, i * 512, func=AF.Sigmoid)
            if debug:
                dump("kvT", kvT[:], [128, S], BF16, [("kvT", t) for t in range(NT)])
                dump("kvtok", kvtok[:], [128, NT, 128], BF16, [("kvtok", t) for t in range(NT)])
                dump("kidxT", kidxT[:], [128, S], BF16, [("kidxT", t) for t in range(4)])
                dump("wabs", wabs[:], [128, NT, 8], F32, [("wabs", t) for t in range(NT)])
                dump("wsgn", wsgn[:], [128, NT, 8], F32, [("wsgn", t) for t in range(NT)])
                dump("xnT", xnT[:], [128, NT, S], BF16, XN_ALL)
            sch.flush()

        if upto >= 3:
          with ExitStack() as st:
            NB = 6
            NIT = 16
            qlT = [sb(st, "qlT%d" % i, [128, 8, 128], BF16) for i in range(NB)]
            qiT = [sb(st, "qiT%d" % i, [128, 4, 128], BF16) for i in range(NB)]
            score = [sb(st, "score%d" % i, [128, S], F32) for i in range(NB)]
            maskb = [sb(st, "maskb%d" % i, [128, S], BF16) for i in range(NB)]
            dsg = [sb(st, "dsg%d" % i, [128, 8, 128], BF16) for i in range(NB)]
            negw = [sb(st, "negw%d" % i, [128, S], F32) for i in range(2)]
            junkc = [sb(st, "junkc%d" % i, [128, S], BF16) for i in range(2)]
            rh = [sb(st, "rh%d" % i, [128, 512], BF16) for i in range(3)]
            m8a = [sb(st, "m8a%d" % i, [128, 8], F32) for i in range(NB)]
            m8n = [sb(st, "m8n%d" % i, [128, 8], F32) for i in range(NB)]
            lo = [sb(st, "lo%d" % i, [128, 1], F32) for i in range(NB)]
            stp = [sb(st, "stp%d" % i, [128, 1], F32) for i in range(NB)]
            mid = [sb(st, "mid%d" % i, [128, 1], F32) for i in range(NB)]
            cnt = [sb(st, "cnt%d" % i, [128, 1], F32) for i in range(NB)]
            tt = [sb(st, "tt%d" % i, [128, 1], F32) for i in range(NB)]
            thr0 = sb(st, "thr0", [128, 1], F32)
            lo2 = [sb(st, "lo2_%d" % i, [128, 2], F32) for i in range(3)]
            stp2 = [sb(st, "stp2_%d" % i, [128, 2], F32) for i in range(3)]
            mid2 = [sb(st, "mid2_%d" % i, [128, 2], F32) for i in range(3)]
            cnt2 = [sb(st, "cnt2_%d" % i, [128, 2], F32) for i in range(3)]
            tt2 = [sb(st, "tt2_%d" % i, [128, 2], F32) for i in range(3)]
            PT = [sb(st, "PT%d" % i, [128, 512], BF16) for i in range(4)]
            rden = sb(st, "rden", [128, 1024], F32)
            oTs = sb(st, "oTs", [128, 1024], BF16)
            osb = [sb(st, "osb%d" % i, [128, 1024], F32) for i in range(4)]
            dsb = [sb(st, "dsb%d" % i, [128, 1024], F32) for i in range(4)]
            oaS = [sb(st, "oaS%d" % i, [128, 8, 128], BF16) for i in range(2)]
            wuv = sb(st, "wuv", [128, 8, 128], BF16)
            psI = [ps(st, "psI%d" % i, [128, 512], F32) for i in range(2)]
            psSc = [ps(st, "psSc%d" % i, [128, 512], F32) for i in range(2)]
            psS = [ps(st, "psS%d" % i, [128, 512], F32) for i in range(2)]
            psO = ps(st, "psO", [128, 512], F32)
            psD = ps(st, "psD", [128, 512], F32)
            ATT_SCALE = 128.0 ** -0.5
            add("pool", lambda e: e.dma_start(out=wuv[:], in_=w_uv.rearrange("h c d -> c h d")), writes=["wuv"], dma=True)
            add("dve", lambda e: e.memset(thr0[:], -1.0e29), writes=["thr0"])
            ident8 = cbs("ident8")
            ones_b = cbs("ones")
            icnt = [0]
            rcnt = [0]
            sccnt = [0]
            ptc = [0]
            scc = [0]

            def dsa_load(i):
                b = i % NB
                add("sp", lambda e: e.dma_start(out=qlT[b][:], in_=qlatT_d[:, i * 128:(i + 1) * 128].rearrange("(h c) q -> c h q", h=8)),
                    reads=[(id(qlatT_d), 0, j) for j in range(4)] + [(id(qlatT_d), 512, j) for j in range(4)],
                    writes=[("qlT", b)], dma=True)
                add("sp", lambda e: e.dma_start(out=qiT[b][:], in_=qidxT_d[:, i * 128:(i + 1) * 128].rearrange("(j r) q -> r j q", j=4)),
                    reads=[(id(qidxT_d), 0, j) for j in range(4)], writes=[("qiT", b)], dma=True)

            def dsa_index(i):
                b = i % NB
                nk = (i + 1) * 128
                nblk = (nk + 511) // 512
                add("dve", lambda e: e.tensor_tensor(out=dsg[b][:], in0=ident.unsqueeze(1).to_broadcast([128, 8, 128]),
                                                     in1=wsgn[:, i, :].unsqueeze(2).to_broadcast([128, 8, 128]), op=ALU.mult),
                    reads=["cb", ("wsgn", i)], writes=[("dsg", b)])
                seq = [(kb, h) for kb in range(nblk) for h in range(8)]
                info = {}

                def dots(idx):
                    kb, h = seq[idx]
                    width = min(512, nk - kb * 512)
                    pb = icnt[0] % 2
                    icnt[0] += 1
                    r = rcnt[0] % 3
                    rcnt[0] += 1
                    r0 = (h % 2) * 64
                    info[idx] = (pb, r, width)
                    add("pe", lambda e: e.matmul(psI[pb][:, 0:width], lhsT=qiT[b][r0:r0 + 64, h // 2, :],
                                                 rhs=kidxT[r0:r0 + 64, kb * 512:kb * 512 + width], start=True, stop=True),
                        reads=[("qiT", b)] + [("kidxT", q) for q in range(4)], writes=[("psI", pb)])
                    add("act", lambda e: e.activation(out=rh[r][:, 0:width], in_=psI[pb][:, 0:width], func=AF.Relu, scale=wabs[:, i, h:h + 1]),
                        reads=[("psI", pb), ("wabs", i)], writes=[("rh", r)])

                def acc(idx):
                    kb, h = seq[idx]
                    pb, r, width = info[idx]
                    if h == 0:
                        scc[0] = sccnt[0] % 2
                        sccnt[0] += 1
                    sc = scc[0]
                    add("pe", lambda e: e.matmul(psSc[sc][:, 0:width], lhsT=dsg[b][:, h, :], rhs=rh[r][:, 0:width], start=(h == 0), stop=(h == 7)),
                        reads=[("dsg", b), ("rh", r)], writes=[("psSc", sc)])
                    if h == 7:
                        add("act", lambda e: e.activation(out=score[b][:, kb * 512:kb * 512 + width], in_=psSc[sc][:, 0:width], func=AF.Copy),
                            reads=[("psSc", sc)], writes=[("score", b, kb)])
                dots(0)
                for idx in range(len(seq)):
                    if idx + 1 < len(seq):
                        dots(idx + 1)
                    acc(idx)
                    if idx % 2 == 1:
                        yield
                yield

            def dsa_topk_pair(i0_):
                tiles = (i0_, i0_ + 1)
                pb_ = (i0_ // 2) % 3
                for i in tiles:
                    b = i % NB
                    nk = (i + 1) * 128
                    add("dve", lambda e, b=b, nk=nk: e.memset(score[b][0:64, nk - 64:nk], NEG),
                        reads=[("score", b, (nk - 1) // 512)], writes=[("score", b, (nk - 1) // 512)])
                if i0_ < 2:
                    for i in tiles:
                        b = i % NB
                        nk = (i + 1) * 128
                        skeys = [("score", b, kb) for kb in range((nk + 511) // 512)]
                        add("dve", lambda e, b=b, nk=nk: e.tensor_scalar(out=maskb[b][:, 0:nk], in0=score[b][:, 0:nk], scalar1=thr0[:, 0:1], scalar2=-MBIG,
                                                                         op0=ALU.is_lt, op1=ALU.mult),
                            reads=skeys + ["thr0"], writes=[("maskb", b)])
                    return
                L2, S2, M2, C2, T2 = lo2[pb_], stp2[pb_], mid2[pb_], cnt2[pb_], tt2[pb_]
                kL, kS, kM, kC, kT = ("lo2", pb_), ("stp2", pb_), ("mid2", pb_), ("cnt2", pb_), ("tt2", pb_)
                info = []
                for j, i in enumerate(tiles):
                    b = i % NB
                    c = i % 2
                    nk = (i + 1) * 128
                    skeys = [("score", b, kb) for kb in range((nk + 511) // 512)]
                    info.append((j, b, c, nk, skeys))
                for j, b, c, nk, skeys in info:
                    add("dve", lambda e, b=b, nk=nk: e.max(out=m8a[b][:], in_=score[b][:, 0:nk]), reads=skeys, writes=[("m8a", b)])
                    add("dve", lambda e, b=b, c=c, nk=nk: e.tensor_scalar(out=negw[c][:, 0:nk], in0=score[b][:, 0:nk], scalar1=-1.0, scalar2=None, op0=ALU.mult),
                        reads=skeys, writes=[("negw", c)])
                for j, b, c, nk, skeys in info:
                    add("dve", lambda e, c=c, nk=nk: e.memset(negw[c][0:64, nk - 64:nk], NEG), reads=[("negw", c)], writes=[("negw", c)])
                    add("dve", lambda e, b=b, c=c, nk=nk: e.max(out=m8n[b][:], in_=negw[c][:, 0:nk]), reads=[("negw", c)], writes=[("m8n", b)])
                for j, b, c, nk, skeys in info:
                    add("dve", lambda e, j=j, b=b: e.tensor_scalar(out=L2[:, j:j + 1], in0=m8n[b][:, 0:1], scalar1=-1.0, scalar2=None, op0=ALU.mult),
                        reads=[("m8n", b)], writes=[kL])
                    add("dve", lambda e, j=j, b=b: e.tensor_tensor(out=S2[:, j:j + 1], in0=m8a[b][:, 0:1], in1=m8n[b][:, 0:1], op=ALU.add),
                        reads=[("m8a", b), ("m8n", b)], writes=[kS])
                add("dve", lambda e: e.tensor_scalar(out=S2[:], in0=S2[:], scalar1=0.5, scalar2=None, op0=ALU.mult), reads=[kS], writes=[kS])
                for k in range(NIT):
                    add("dve", lambda e: e.tensor_tensor(out=M2[:], in0=L2[:], in1=S2[:], op=ALU.add), reads=[kL, kS], writes=[kM])
                    for j, b, c, nk, skeys in info:
                        add("dve", lambda e, j=j, b=b, c=c, nk=nk: e.tensor_scalar(
                            out=junkc[c][:, 0:nk], in0=score[b][:, 0:nk], scalar1=M2[:, j:j + 1], scalar2=0.0,
                            op0=ALU.is_ge, op1=ALU.add, accum_out=C2[:, j:j + 1]),
                            reads=skeys + [kM], writes=[("junkc", c), (kC, j)])
                    add("dve", lambda e: e.scalar_tensor_tensor(out=T2[:], in0=C2[:], scalar=256.0, in1=S2[:], op0=ALU.is_ge, op1=ALU.mult),
                        reads=[(kC, 0), (kC, 1), kS], writes=[kT])
                    add("dve", lambda e: e.tensor_tensor(out=L2[:], in0=L2[:], in1=T2[:], op=ALU.add), reads=[kL, kT], writes=[kL])
                    add("dve", lambda e: e.tensor_scalar(out=S2[:], in0=S2[:], scalar1=0.5, scalar2=None, op0=ALU.mult), reads=[kS, kT], writes=[kS])
                for j, b, c, nk, skeys in info:
                    add("dve", lambda e, j=j, b=b, nk=nk: e.tensor_scalar(out=maskb[b][:, 0:nk], in0=score[b][:, 0:nk], scalar1=L2[:, j:j + 1], scalar2=-MBIG,
                                                                          op0=ALU.is_lt, op1=ALU.mult),
                        reads=skeys + [kL], writes=[("maskb", b)])

            def dsa_attend(i):
                b = i % NB
                ob_ = i % 2
                for hf in range(2):
                    pis = {}

                    def fS_add(kt, hf=hf):
                        def fS(e):
                            e.matmul(psS[kt % 2][:, :], lhsT=kvT[:, kt * 128:(kt + 1) * 128], rhs=qlT[b][:, hf * 4:(hf + 1) * 4, :],
                                     start=True, stop=False)
                            return e.matmul(psS[kt % 2][:, :], lhsT=maskb[b][:, kt * 128:(kt + 1) * 128], rhs=ident8[:, 0:512],
                                            start=False, stop=True)
                        add("pe", fS, reads=[("kvT", kt), ("qlT", b), ("maskb", b), "cb"], writes=[("psS", kt % 2)])
                    fS_add(0)
                    for kt in range(i + 1):
                        if kt + 1 <= i:
                            fS_add(kt + 1)
                        pi = ptc[0] % 4
                        ptc[0] += 1
                        add("act", lambda e, kt=kt, pi=pi: e.activation(out=PT[pi][:], in_=psS[kt % 2][:, :], func=AF.Exp, scale=ATT_SCALE),
                            reads=[("psS", kt % 2)], writes=[("PT", pi)])

                        def fO(e, kt=kt, pi=pi):
                            e.matmul(psO[:, :], lhsT=kvtok[:, kt, :], rhs=PT[pi][:], start=(kt == 0), stop=(kt == i))
                            return e.matmul(psD[:, :], lhsT=ones_b, rhs=PT[pi][:], start=(kt == 0), stop=(kt == i))
                        add("pe", fO, reads=[("kvtok", kt), ("PT", pi), "cb"], writes=["psO", "psD"])
                    add("act", lambda e, hf=hf: e.activation(out=dsb[i % 4][:, hf * 512:(hf + 1) * 512], in_=psD[:, :], func=AF.Copy),
                        reads=["psD"], writes=[("dsb", i % 4, hf)])
                    add("act", lambda e, hf=hf: e.activation(out=osb[i % 4][:, hf * 512:(hf + 1) * 512], in_=psO[:, :], func=AF.Copy),
                        reads=["psO"], writes=[("osb", i % 4, hf)])
                yield

            def dsa_finish(i):
                b = i % NB
                ob_ = i % 2
                for hf in range(2):
                    add("dve", lambda e, hf=hf: e.reciprocal(out=rden[:, hf * 512:(hf + 1) * 512], in_=dsb[i % 4][:, hf * 512:(hf + 1) * 512]),
                        reads=[("dsb", i % 4, hf)], writes=[("rden", hf)])
                    add("dve", lambda e, hf=hf: e.tensor_tensor(out=oTs[:, hf * 512:(hf + 1) * 512], in0=osb[i % 4][:, hf * 512:(hf + 1) * 512],
                                                                in1=rden[:, hf * 512:(hf + 1) * 512], op=ALU.mult),
                        reads=[("osb", i % 4, hf), ("rden", hf)], writes=[("oTs", hf)])
                for hf in range(2):
                    def fU(e, hf=hf):
                        ins = None
                        for hh in range(4):
                            h = hf * 4 + hh
                            ins = e.matmul(psI[hf][:, hh * 128:(hh + 1) * 128], lhsT=wuv[:, h, :], rhs=oTs[:, h * 128:(h + 1) * 128],
                                           start=True, stop=True)
                        return ins
                    add("pe", fU, reads=["wuv", ("oTs", hf)], writes=[("psI", hf)])
                    add("act", lambda e, hf=hf: e.activation(out=oaS[ob_][:, hf * 4:(hf + 1) * 4, :],
                                                             in_=psI[hf][:, :].rearrange("p (h q) -> p h q", h=4), func=AF.Copy),
                        reads=[("psI", hf)], writes=[("oaS", ob_, hf)])
                add("sp", lambda e: e.dma_start(out=oaT_d[:, i * 128:(i + 1) * 128].rearrange("(h d) q -> d h q", h=8), in_=oaS[ob_][:]),
                    reads=[("oaS", ob_, 0), ("oaS", ob_, 1)], writes=[("oaT_d", i)], dma=True)
                yield

            def chain(*gens):
                for g in gens:
                    yield from g

            NTD = int(os.environ.get("DSA_TILES", NT))
            NPAIR = NTD // 2
            for step in range(NPAIR + 3):
                pi_ = step
                pt_ = step - 1
                pa_ = step - 2
                pf_ = step - 3
                conv(2)
                if pi_ < NPAIR:
                    dsa_load(2 * pi_)
                    dsa_load(2 * pi_ + 1)
                if 0 <= pf_ < NPAIR:
                    for i in (2 * pf_, 2 * pf_ + 1):
                        for _ in dsa_finish(i):
                            pass
                if pi_ < NPAIR:
                    for i in (2 * pi_, 2 * pi_ + 1):
                        for _ in dsa_index(i):
                            pass
                if 0 <= pt_ < NPAIR:
                    dsa_topk_pair(2 * pt_)
                if 0 <= pa_ < NPAIR:
                    for i in (2 * pa_, 2 * pa_ + 1):
                        for _ in dsa_attend(i):
                            pass
            sch.flush()

        KC.close()
        if upto >= 4:
          with ExitStack() as st:
            qr = [sb(st, "qr%d" % i, [128, 1024], BF16) for i in range(2)]
            kr = [sb(st, "kr%d" % i, [128, 1024], BF16) for i in range(2)]
            kd = [sb(st, "kd%d" % i, [128, 1024], BF16) for i in range(2)]
            vr = [sb(st, "vr%d" % i, [128, 1024], BF16) for i in range(2)]
            gr = [sb(st, "gr%d" % i, [128, 1024], BF16) for i in range(2)]
            qdT = sb(st, "qdT", [128, 1024], BF16)
            kTs = sb(st, "kTs", [128, 1024], BF16)
            ATs = sb(st, "ATs", [128, 1024], BF16)
            gkm = sb(st, "gkm", [128, 8, 128], F32)
            Sf = sb(st, "Sf", [128, 1024], F32)
            Sb = sb(st, "Sb", [128, 1024], BF16)
            osq = sb(st, "osq", [128, 1024], F32)
            oc = sb(st, "oc", [128, 1024], F32)
            ob = sb(st, "ob", [128, 1024], BF16)
            obS = [sb(st, "obS%d" % i, [128, 8, 128], BF16) for i in range(2)]
            gretbc = sb(st, "gretbc", [128, 1024], F32)
            st8 = sb(st, "st8", [128, 8], F32)
            sq8 = sb(st, "sq8", [128, 8], F32)
            mean8 = sb(st, "mean8", [128, 8], F32)
            var8 = sb(st, "var8", [128, 8], F32)
            rs8 = sb(st, "rs8", [128, 8], F32)
            pTq = ps(st, "pTq", [128, 1024], BF16)
            pTk = ps(st, "pTk", [128, 1024], BF16)
            psA = [ps(st, "psA%d" % i, [128, 512], F32) for i in range(2)]
            psR = [ps(st, "psR%d" % i, [128, 512], F32) for i in range(2)]
            psT = [ps(st, "psT%d" % i, [128, 512], F32) for i in range(2)]
            add("sp", lambda e: e.dma_start(out=gretbc[:], in_=g_ret[0:1, :].to_broadcast([128, 1024])), writes=["gretbc"], dma=True)
            for h in range(8):
                add("dve", lambda e, h=h: e.tensor_scalar(out=gkm[:, h, :], in0=cfs("cmask"), scalar1=cfs("gk")[:, h:h + 1], scalar2=None, op0=ALU.mult),
                    reads=["cf"], writes=["gkm"])
            add("dve", lambda e: e.memset(Sf[:], 0.0), writes=["Sf"])
            dqt = cfs("dq")

            def ret_load(t):
                b = t % 2
                rows = slice(t * 128, (t + 1) * 128)
                for name, dst, src in (("qr", qr, qrot_d), ("kr", kr, krot_d), ("kd", kd, kdec_d), ("vr", vr, vr_d), ("gr", gr, gr_d)):
                    rk = [("kdec_d", c0, t // 4) for c0 in (0, 512)] if name == "kd" else [(id(src), c0, t // 4) for c0 in (0, 512)]
                    add("sp", lambda e, dst=dst, src=src: e.dma_start(out=dst[b][:], in_=src[rows, :]), reads=rk, writes=[(name, b)], dma=True)

            NTR = int(os.environ.get("RET_TILES", NT))
            RP = int(os.environ.get("RET_PART", 9))
            ret_load(0)
            for t in range(NTR):
                b = t % 2
                if t + 1 < NTR:
                    ret_load(t + 1)

                def fT(e, b=b):
                    ins = None
                    for h in range(8):
                        e.transpose(out=pTq[:, h * 128:(h + 1) * 128], in_=qr[b][:, h * 128:(h + 1) * 128], identity=ident)
                        ins = e.transpose(out=pTk[:, h * 128:(h + 1) * 128], in_=kr[b][:, h * 128:(h + 1) * 128], identity=ident)
                    return ins
                add("pe", fT, reads=[("qr", b), ("kr", b), "cb"], writes=["pTq", "pTk"])
                add("dve", lambda e: e.tensor_tensor(out=qdT[:], in0=pTq[:, :], in1=dqt, op=ALU.mult), reads=["pTq", "cf"], writes=["qdT"])
                add("act", lambda e: e.activation(out=kTs[:], in_=pTk[:, :], func=AF.Copy), reads=["pTk"], writes=["kTs"])
                for hf in range(2):
                    def fA(e, hf=hf):
                        ins = None
                        for hh in range(4):
                            h = hf * 4 + hh
                            ins = e.matmul(psA[hf][:, hh * 128:(hh + 1) * 128], lhsT=kTs[:, h * 128:(h + 1) * 128], rhs=qdT[:, h * 128:(h + 1) * 128],
                                           start=True, stop=True)
                        return ins
                    add("pe", fA, reads=["kTs", "qdT"], writes=[("psA", hf)])
                    add("dve", lambda e, hf=hf: e.tensor_tensor(out=ATs[:, hf * 512:(hf + 1) * 512], in0=psA[hf][:, :],
                                                                in1=gkm[:, hf * 4:(hf + 1) * 4, :].rearrange("p h n -> p (h n)"), op=ALU.mult),
                        reads=[("psA", hf), "gkm"], writes=[("ATs", hf)])
                for hf in range(2):
                    def fR(e, hf=hf, t=t, b=b):
                        ins = None
                        for hh in range(4):
                            h = hf * 4 + hh
                            ins = e.matmul(psR[hf][:, hh * 128:(hh + 1) * 128], lhsT=ATs[:, h * 128:(h + 1) * 128], rhs=vr[b][:, h * 128:(h + 1) * 128],
                                           start=True, stop=(t == 0))
                            if t > 0:
                                ins = e.matmul(psR[hf][:, hh * 128:(hh + 1) * 128], lhsT=qdT[:, h * 128:(h + 1) * 128], rhs=Sb[:, h * 128:(h + 1) * 128],
                                               start=False, stop=True)
                        return ins
                    add("pe", fR, reads=[("ATs", hf), ("vr", b), "qdT", "Sb"], writes=[("psR", hf)])
                for hf in range(2):
                    def fS2(e, hf=hf, b=b):
                        ins = None
                        for hh in range(4):
                            h = hf * 4 + hh
                            ins = e.matmul(psT[hf][:, hh * 128:(hh + 1) * 128], lhsT=kd[b][:, h * 128:(h + 1) * 128], rhs=vr[b][:, h * 128:(h + 1) * 128],
                                           start=True, stop=True)
                        return ins
                    add("pe", fS2, reads=[("kd", b), ("vr", b)], writes=[("psT", hf)])
                if RP < 2:
                    continue
                for h in range(8):
                    hsl = slice(h * 128, (h + 1) * 128)
                    psl = psR[h // 4][:, (h % 4) * 128:(h % 4 + 1) * 128]
                    add("act", lambda e, h=h, hsl=hsl, psl=psl: e.activation(out=oc[:, hsl], in_=psl, func=AF.Copy, accum_out=st8[:, h:h + 1]),
                        reads=[("psR", h // 4)], writes=[("oc", h // 4), ("st8", h // 4)])
                    add("act", lambda e, h=h, hsl=hsl, psl=psl: e.activation(out=osq[:, hsl], in_=psl, func=AF.Square, accum_out=sq8[:, h:h + 1]),
                        reads=[("psR", h // 4)], writes=[("osq", h // 4), ("sq8", h // 4)])
                if RP < 3:
                    continue
                add("dve", lambda e: e.tensor_scalar(out=mean8[:], in0=st8[:], scalar1=1.0 / 128, scalar2=None, op0=ALU.mult),
                    reads=[("st8", 0), ("st8", 1)], writes=["mean8"])
                add("dve", lambda e: e.tensor_tensor(out=var8[:], in0=mean8[:], in1=mean8[:], op=ALU.mult), reads=["mean8"], writes=["var8"])
                add("dve", lambda e: e.scalar_tensor_tensor(out=var8[:], in0=sq8[:], scalar=1.0 / 128, in1=var8[:], op0=ALU.mult, op1=ALU.subtract),
                    reads=[("sq8", 0), ("sq8", 1), "var8"], writes=["var8"])
                add("act", lambda e: e.activation(out=rs8[:], in_=var8[:], func=AF.Sqrt, scale=1.0, bias=EPS), reads=["var8"], writes=["rs8"])
                add("dve", lambda e: e.reciprocal(out=rs8[:], in_=rs8[:]), reads=["rs8"], writes=["rs8"])
                for hf in range(2):
                    ocv = oc[:, hf * 512:(hf + 1) * 512].rearrange("p (h n) -> p h n", h=4)
                    add("dve", lambda e, hf=hf, ocv=ocv: e.tensor_tensor(out=ocv, in0=ocv,
                                                                         in1=mean8[:, hf * 4:(hf + 1) * 4].unsqueeze(2).to_broadcast([128, 4, 128]), op=ALU.subtract),
                        reads=[("oc", hf), "mean8"], writes=[("oc", hf)])
                    add("dve", lambda e, hf=hf, ocv=ocv: e.tensor_tensor(out=ocv, in0=ocv,
                                                                         in1=rs8[:, hf * 4:(hf + 1) * 4].unsqueeze(2).to_broadcast([128, 4, 128]), op=ALU.mult),
                        reads=[("oc", hf), "rs8"], writes=[("oc", hf)])
                if RP < 4:
                    continue
                add("pool", lambda e: e.tensor_tensor(out=oc[:], in0=oc[:], in1=gretbc[:], op=ALU.mult),
                    reads=[("oc", 0), ("oc", 1), "gretbc"], writes=[("oc", 0), ("oc", 1)])
                add("dve", lambda e, b=b: e.tensor_tensor(out=ob[:], in0=oc[:], in1=gr[b][:], op=ALU.mult),
                    reads=[("oc", 0), ("oc", 1), ("gr", b)], writes=["ob"])
                if RP < 5:
                    continue
                for h in range(8):
                    add("dve", lambda e, h=h: e.scalar_tensor_tensor(out=Sf[:, h * 128:(h + 1) * 128], in0=Sf[:, h * 128:(h + 1) * 128], scalar=_G128[h],
                                                                     in1=psT[h // 4][:, (h % 4) * 128:(h % 4 + 1) * 128], op0=ALU.mult, op1=ALU.add),
                        reads=["Sf", ("psT", h // 4)], writes=["Sf"])
                add("act", lambda e: e.activation(out=Sb[:], in_=Sf[:], func=AF.Copy), reads=["Sf"], writes=["Sb"])

                def fT2(e):
                    ins = None
                    for h in range(8):
                        ins = e.transpose(out=pTq[:, h * 128:(h + 1) * 128], in_=ob[:, h * 128:(h + 1) * 128], identity=ident)
                    return ins
                add("pe", fT2, reads=["ob", "cb"], writes=["pTq"])
                add("act", lambda e, b=b: e.activation(out=obS[b][:], in_=pTq[:, :].rearrange("p (h q) -> p h q", h=8), func=AF.Copy),
                    reads=["pTq"], writes=[("obS", b)])
                add("sp", lambda e, t=t, b=b: e.dma_start(out=obT_d[:, t * 128:(t + 1) * 128].rearrange("(h d) q -> d h q", h=8), in_=obS[b][:]),
                    reads=[("obS", b)], writes=[("obT_d", t)], dma=True)
            sch.flush()

        lg = sb(P, "lg", [128, NT, 36], F32)
        d1i = sb(P, "d1i", [128, NT], I32)
        d2i = sb(P, "d2i", [128, NT], I32)
        g1 = sb(P, "g1", [128, NT], F32)
        g2 = sb(P, "g2", [128, NT], F32)

        if upto >= 5:
          with ExitStack() as st2:
            mixT = sb(st2, "mixT", [128, 16, S], BF16)
            with ExitStack() as st:
                Wb = sb(st, "Wb", [128, 2, 8, D], BF16)
                oaB = sb(st, "oaB", [128, 8, 512], BF16)
                obB = sb(st, "obB", [128, 8, 512], BF16)
                gAB = [sb(st, "gAB%d" % i, [128, 2, 512], BF16) for i in range(3)]
                t1 = [sb(st, "t1_%d" % i, [128, 512], F32) for i in range(2)]
                t2 = [sb(st, "t2_%d" % i, [128, 512], F32) for i in range(2)]
                psB = [ps(st, "psB%d" % i, [128, 512], F32) for i in range(4)]
                for hc in range(2):
                    for n in range(2):
                        add("pool", lambda e, n=n, hc=hc: e.dma_start(
                            out=Wb[:, n, :, hc * 1024:(hc + 1) * 1024],
                            in_=w_branch[n, :, hc * 1024:(hc + 1) * 1024].rearrange("(c p) d -> p c d", p=128)),
                            writes=[("Wb", n, hc)], dma=True)
                WBK = [("Wb", n, hc) for n in range(2) for hc in range(2)]
                gc = 0
                for tb in range(4):
                    cols = slice(tb * 512, (tb + 1) * 512)
                    add("sp", lambda e, cols=cols: e.dma_start(out=oaB[:], in_=oaT_d[:, cols].rearrange("(c p) q -> p c q", p=128)),
                        reads=[("oaT_d", i) for i in range(tb * 4, tb * 4 + 4)], writes=["oaB"], dma=True)
                    add("sp", lambda e, cols=cols: e.dma_start(out=obB[:], in_=obT_d[:, cols].rearrange("(c p) q -> p c q", p=128)),
                        reads=[("obT_d", i) for i in range(tb * 4, tb * 4 + 4)], writes=["obB"], dma=True)
                    for j in range(16):
                        gs = gc % 3
                        pa = (gc % 2) * 2
                        gc += 1
                        add("sp", lambda e, j=j, gs=gs, cols=cols: e.dma_start(
                            out=gAB[gs][:], in_=gbT_d.rearrange("(n r) q -> r n q", n=2)[j * 128:(j + 1) * 128, :, cols]),
                            reads=[(id(gbT_d), (j // 4) * 512, j % 4), (id(gbT_d), 2048 + (j // 4) * 512, j % 4)], writes=[("gAB", gs)], dma=True)
                        for n, src in ((0, oaB), (1, obB)):
                            def fB(e, n=n, src=src, j=j, pa=pa):
                                ins = None
                                for c in range(8):
                                    ins = e.matmul(psB[pa + n][:, :], lhsT=Wb[:, n, c, j * 128:(j + 1) * 128], rhs=src[:, c, :],
                                                   start=(c == 0), stop=(c == 7))
                                return ins
                            add("pe", fB, reads=[("Wb", n, j // 8), "oaB" if n == 0 else "obB"], writes=[("psB", pa + n)])
                        q = (gc % 2)
                        add("dve", lambda e, gs=gs, pa=pa, q=q: e.tensor_tensor(out=t1[q][:], in0=psB[pa][:, :], in1=gAB[gs][:, 0, :], op=ALU.mult),
                            reads=[("psB", pa), ("gAB", gs)], writes=[("t1", q)])
                        add("dve", lambda e, gs=gs, pa=pa, q=q: e.tensor_tensor(out=t2[q][:], in0=psB[pa + 1][:, :], in1=gAB[gs][:, 1, :], op=ALU.mult),
                            reads=[("psB", pa + 1), ("gAB", gs)], writes=[("t2", q)])
                        add("pool", lambda e, j=j, q=q, cols=cols: e.tensor_tensor(out=mixT[:, j, cols], in0=t1[q][:], in1=t2[q][:], op=ALU.add),
                            reads=[("t1", q), ("t2", q)], writes=[("mixT", tb)])
                sch.flush()

            with ExitStack() as st:
                Wo = sb(st, "Wo", [128, 16, D], BF16)
                Wr = sb(st, "Wr", [128, 16, 36], BF16)
                brbc = sb(st, "brbc", [128, 36], F32)
                gfbc = sb(st, "gfbc", [128, D], F32)
                xt2 = [sb(st, "xt2_%d" % i, [128, D], F32) for i in range(2)]
                h1t = [sb(st, "h1t%d" % i, [128, D], F32) for i in range(2)]
                xn2 = [sb(st, "xn2_%d" % i, [128, D], BF16) for i in range(2)]
                xn2T = sb(st, "xn2T", [128, 16, 128], BF16)
                junk3 = sb(st, "junk3", [128, D], BF16)
                ss2 = sb(st, "ss2", [128, NT], F32)
                rs2 = sb(st, "rs2", [128, NT], F32)
                psH = [ps(st, "psH%d" % i, [128, 512], F32) for i in range(4)]
                pT2 = [ps(st, "pT2_%d" % i, [128, 1024], BF16) for i in range(2)]
                psL = ps(st, "psL", [128, 512], F32)
                for hc in range(4):
                    add("pool", lambda e, hc=hc: e.dma_start(out=Wo[:, :, hc * 512:(hc + 1) * 512],
                                                             in_=w_out[:, hc * 512:(hc + 1) * 512].rearrange("(c p) d -> p c d", p=128)),
                        writes=[("Wo", hc)], dma=True)
                add("pool", lambda e: e.dma_start(out=Wr[:, :, 0:4], in_=w_rg.rearrange("(c p) g -> p c g", p=128)), writes=["Wr"], dma=True)
                add("pool", lambda e: e.dma_start(out=Wr[:, :, 4:36], in_=w_re.rearrange("(c p) g -> p c g", p=128)), writes=["Wr"], dma=True)
                add("sp", lambda e: e.dma_start(out=brbc[:, 0:4], in_=b_rg[0:1, :].to_broadcast([128, 4])), writes=["brbc"], dma=True)
                add("sp", lambda e: e.dma_start(out=brbc[:, 4:36], in_=b_re[0:1, :].to_broadcast([128, 32])), writes=["brbc"], dma=True)
                add("sp", lambda e: e.dma_start(out=gfbc[:], in_=g_ffn[0:1, :].to_broadcast([128, D])), writes=["gfbc"], dma=True)
                hc_ = [0]

                def e2_front(t):
                    b = t % 2
                    rows = slice(t * 128, (t + 1) * 128)
                    add("sp", lambda e, b=b, rows=rows: e.dma_start(out=xt2[b][:], in_=x[rows, :]), writes=[("xt2", b)], dma=True)
                    for cbk in range(4):
                        pb = hc_[0] % 4
                        hc_[0] += 1

                        def fH(e, t=t, cbk=cbk, pb=pb):
                            ins = None
                            for j in range(16):
                                ins = e.matmul(psH[pb][:, :], lhsT=mixT[:, j, t * 128:(t + 1) * 128], rhs=Wo[:, j, cbk * 512:(cbk + 1) * 512],
                                               start=(j == 0), stop=(j == 15))
                            return ins
                        add("pe", fH, reads=[("mixT", t // 4), ("Wo", cbk)], writes=[("psH", pb)])
                        add("dve", lambda e, b=b, cbk=cbk, pb=pb: e.tensor_tensor(out=h1t[b][:, cbk * 512:(cbk + 1) * 512], in0=psH[pb][:, :],
                                                                                  in1=xt2[b][:, cbk * 512:(cbk + 1) * 512], op=ALU.add),
                            reads=[("psH", pb), ("xt2", b)], writes=[("h1t", b, cbk)])

                def e2_back(t):
                    b = t % 2
                    rows = slice(t * 128, (t + 1) * 128)
                    conv(1)
                    HK = [("h1t", b, c) for c in range(4)]
                    add("sp", lambda e, b=b, rows=rows: e.dma_start(out=h1_d[rows, :], in_=h1t[b][:]), reads=HK, writes=[("h1_d", t)], dma=True)
                    add("act", lambda e, t=t, b=b: e.activation(out=junk3[:], in_=h1t[b][:], func=AF.Square, accum_out=ss2[:, t:t + 1]),
                        reads=HK, writes=["junk3", ("ss2", t)])
                    add("act", lambda e, t=t: e.activation(out=rs2[:, t:t + 1], in_=ss2[:, t:t + 1], func=AF.Sqrt, scale=1.0 / D, bias=EPS),
                        reads=[("ss2", t)], writes=[("rs2", t)])
                    add("dve", lambda e, t=t: e.reciprocal(out=rs2[:, t:t + 1], in_=rs2[:, t:t + 1]), reads=[("rs2", t)], writes=[("rs2", t)])
                    add("dve", lambda e, t=t, b=b: e.scalar_tensor_tensor(out=xn2[b][:], in0=h1t[b][:], scalar=rs2[:, t:t + 1], in1=gfbc[:],
                                                                          op0=ALU.mult, op1=ALU.mult),
                        reads=HK + [("rs2", t), "gfbc"], writes=[("xn2", b)])
                    add("sp", lambda e, b=b, rows=rows: e.dma_start(out=xn2_d[rows, :], in_=xn2[b][:]), reads=[("xn2", b)], writes=[("xn2_d", t)], dma=True)

                    def tr2(e, b=b):
                        ins = None
                        for c in range(16):
                            ins = e.transpose(out=pT2[c // 8][:, (c % 8) * 128:(c % 8 + 1) * 128], in_=xn2[b][:, c * 128:(c + 1) * 128], identity=ident)
                        return ins
                    add("pe", tr2, reads=[("xn2", b), "cb"], writes=["pT2a", "pT2b"])
                    add("act", lambda e: e.activation(out=xn2T[:, 0:8, :], in_=pT2[0][:].rearrange("p (c n) -> p c n", c=8), func=AF.Copy),
                        reads=["pT2a"], writes=["xn2Ta"])
                    add("dve", lambda e: e.tensor_copy(out=xn2T[:, 8:16, :], in_=pT2[1][:].rearrange("p (c n) -> p c n", c=8)),
                        reads=["pT2b"], writes=["xn2Tb"])

                    def fL(e):
                        ins = None
                        for c in range(16):
                            ins = e.matmul(psL[:, 0:36], lhsT=xn2T[:, c, :], rhs=Wr[:, c, :], start=(c == 0), stop=(c == 15))
                        return ins
                    add("pe", fL, reads=["xn2Ta", "xn2Tb", "Wr"], writes=["psL"])
                    add("dve", lambda e, t=t: e.tensor_tensor(out=lg[:, t, :], in0=psL[:, 0:36], in1=brbc[:], op=ALU.add),
                        reads=["psL", "brbc"], writes=[("lg", t)])

                e2_front(0)
                for t in range(NT):
                    if t + 1 < NT:
                        e2_front(t + 1)
                    e2_back(t)
                sch.flush()

        GW = ExitStack()
        NU = 6
        wu = [sb(GW, "wu%d" % i, [128, 16 * 512], BF16) for i in range(NU)]
        uc = [0]

        def load_unit(src, is_down, half, rd=()):
            u = uc[0] % NU
            uc[0] += 1
            if not is_down:
                add("pool", lambda e: e.dma_start(out=wu[u][:].rearrange("p (c f) -> p c f", c=16),
                                                  in_=src[:, half * 512:(half + 1) * 512].rearrange("(c p) f -> p c f", p=128)),
                    reads=list(rd), writes=[("wu", u)], dma=True)
            else:
                add("pool", lambda e: e.dma_start(out=wu[u][:].rearrange("p (c f) -> p c f", c=8),
                                                  in_=src[:, half * 1024:(half + 1) * 1024].rearrange("(c p) f -> p c f", p=128)),
                    reads=list(rd), writes=[("wu", u)], dma=True)
            return u

        conv(1000)
        preloaded = {}
        if upto >= 7:
            for key, src, dn, half in (("g0", w_eg[0], False, 0), ("u0", w_eu[0], False, 0), ("g1", w_eg[0], False, 1),
                                       ("u1", w_eu[0], False, 1), ("d0", w_ed[0], True, 0), ("d1", w_ed[0], True, 1)):
                preloaded[(0, key)] = load_unit(src, dn, half)

        def get_unit(ex, key, src, dn, half):
            if (ex, key) in preloaded:
                return preloaded.pop((ex, key))
            if ex >= NF32:
                kind = key[0]
                src = {"g": wgb_d, "u": wub_d, "d": wdb_d}[kind][ex - NF32]
                return load_unit(src, dn, half, rd=[("wconv", ex, kind)])
            return load_unit(src, dn, half)

        if upto >= 6:
          with ExitStack() as st:
            gmax = sb(st, "gmax", [128, NT], F32)
            eg = sb(st, "eg", [128, NT, 4], F32)
            gsum = sb(st, "gsum", [128, NT], F32)
            pgrp = sb(st, "pgrp", [128, NT], F32)
            pen = sb(st, "pen", [128, NT, 4], F32)
            lm = sb(st, "lm", [128, NT, 32], F32)
            m8r = sb(st, "m8r", [128, NT, 8], F32)
            oh1 = sb(st, "oh1", [128, NT, 32], F32)
            oh2 = sb(st, "oh2", [128, NT, 32], F32)
            ohb = sb(st, "ohb", [128, NT, 32], BF16)
            dl = sb(st, "dl", [128, NT], F32)
            sg = sb(st, "sg", [128, NT], F32)
            tmpc = sb(st, "tmpc", [128, NT, 32], F32)
            tmpd = sb(st, "tmpd", [128, NT, 32], F32)
            d1f = sb(st, "d1f", [128, NT], F32)
            d2f = sb(st, "d2f", [128, NT], F32)
            xg = [sb(st, "xg%d" % i, [128, D], BF16) for i in range(2)]
            psC = ps(st, "psC", [128, 512], F32)
            LG = [("lg", t) for t in range(NT)]
            add("dve", lambda e: e.tensor_tensor(out=eg[:, :, 0:2], in0=lg[:, :, 0:2], in1=lg[:, :, 2:4], op=ALU.max), reads=LG, writes=["eg"])
            add("dve", lambda e: e.tensor_tensor(out=gmax[:].unsqueeze(2), in0=eg[:, :, 0:1], in1=eg[:, :, 1:2], op=ALU.max), reads=["eg"], writes=["gmax"])
            add("dve", lambda e: e.tensor_tensor(out=eg[:], in0=lg[:, :, 0:4], in1=gmax[:].unsqueeze(2).to_broadcast([128, NT, 4]), op=ALU.subtract),
                reads=LG + ["gmax"], writes=["eg"])
            add("dve", lambda e: e.tensor_scalar(out=pen[:], in0=eg[:], scalar1=0.0, scalar2=-1.0e30, op0=ALU.is_lt, op1=ALU.mult),
                reads=["eg"], writes=["pen"])
            add("act", lambda e: e.activation(out=eg[:], in_=eg[:], func=AF.Exp), reads=["eg"], writes=["eg"])
            add("dve", lambda e: e.tensor_tensor(out=pgrp[:].unsqueeze(2), in0=eg[:, :, 0:1], in1=eg[:, :, 1:2], op=ALU.add), reads=["eg"], writes=["pgrp"])
            add("dve", lambda e: e.tensor_tensor(out=gsum[:].unsqueeze(2), in0=eg[:, :, 2:3], in1=eg[:, :, 3:4], op=ALU.add), reads=["eg"], writes=["gsum"])
            add("dve", lambda e: e.tensor_tensor(out=gsum[:], in0=gsum[:], in1=pgrp[:], op=ALU.add), reads=["gsum", "pgrp"], writes=["gsum"])
            add("dve", lambda e: e.reciprocal(out=pgrp[:], in_=gsum[:]), reads=["gsum"], writes=["pgrp"])
            add("dve", lambda e: e.tensor_tensor(out=lm[:].rearrange("p t (g k) -> p t g k", g=4),
                                                 in0=lg[:, :, 4:36].rearrange("p t (g k) -> p t g k", g=4),
                                                 in1=pen[:].unsqueeze(3).to_broadcast([128, NT, 4, 8]), op=ALU.add),
                reads=LG + ["pen"], writes=["lm"])
            for t in range(NT):
                add("dve", lambda e, t=t: e.max(out=m8r[:, t, :], in_=lm[:, t, :]), reads=["lm"], writes=["m8r"])
            add("dve", lambda e: e.tensor_tensor(out=oh1[:], in0=lm[:], in1=m8r[:, :, 0:1].to_broadcast([128, NT, 32]), op=ALU.is_equal),
                reads=["lm", "m8r"], writes=["oh1"])
            add("dve", lambda e: e.tensor_tensor(out=oh2[:], in0=lm[:], in1=m8r[:, :, 1:2].to_broadcast([128, NT, 32]), op=ALU.is_equal),
                reads=["lm", "m8r"], writes=["oh2"])
            add("dve", lambda e: e.tensor_tensor(out=ohb[:], in0=oh1[:], in1=oh2[:], op=ALU.add), reads=["oh1", "oh2"], writes=["ohb"])
            add("dve", lambda e: e.tensor_tensor(out=dl[:].unsqueeze(2), in0=m8r[:, :, 0:1], in1=m8r[:, :, 1:2], op=ALU.subtract), reads=["m8r"], writes=["dl"])
            add("act", lambda e: e.activation(out=sg[:], in_=dl[:], func=AF.Sigmoid), reads=["dl"], writes=["sg"])
            add("dve", lambda e: e.tensor_tensor(out=g1[:], in0=sg[:], in1=pgrp[:], op=ALU.mult), reads=["sg", "pgrp"], writes=["g1"])
            add("dve", lambda e: e.tensor_tensor(out=g2[:], in0=pgrp[:], in1=g1[:], op=ALU.subtract), reads=["g1", "pgrp"], writes=["g2"])
            ones_b = cbs("ones")
            utri = cbs("utri")
            for t in range(NT):
                def fC(e, t=t):
                    ins = None
                    for tp in range(t):
                        ins = e.matmul(psC[:, t * 32:(t + 1) * 32], lhsT=ones_b, rhs=ohb[:, tp, :], start=(tp == 0), stop=False)
                    return e.matmul(psC[:, t * 32:(t + 1) * 32], lhsT=utri, rhs=ohb[:, t, :], start=(t == 0), stop=True)
                add("pe", fC, reads=["ohb", "cb"], writes=["psC"])
            add("dve", lambda e: e.tensor_tensor(out=tmpc[:], in0=psC[:, :].rearrange("p (t k) -> p t k", t=NT),
                                                 in1=cfs("ebase").unsqueeze(1).to_broadcast([128, NT, 32]), op=ALU.add),
                reads=["psC", "cf"], writes=["tmpc"])
            for ohx, dxf, dxi, nm in ((oh1, d1f, d1i, "d1"), (oh2, d2f, d2i, "d2")):
                add("dve", lambda e, ohx=ohx: e.tensor_tensor(out=tmpd[:], in0=tmpc[:], in1=ohx[:], op=ALU.mult),
                    reads=["tmpc", "oh1", "oh2"], writes=["tmpd"])
                for w_ in (16, 8, 4, 2, 1):
                    add("dve", lambda e, w_=w_: e.tensor_tensor(out=tmpd[:, :, 0:w_], in0=tmpd[:, :, 0:w_], in1=tmpd[:, :, w_:2 * w_], op=ALU.add),
                        reads=["tmpd"], writes=["tmpd"])
                add("dve", lambda e, dxf=dxf: e.tensor_copy(out=dxf[:].unsqueeze(2), in_=tmpd[:, :, 0:1]), reads=["tmpd"], writes=[nm + "f"])
                add("dve", lambda e, dxf=dxf, dxi=dxi: e.tensor_copy(out=dxi[:], in_=dxf[:]), reads=[nm + "f"], writes=[nm + "i"])
            if debug:
                dump("lg", lg[:], [128, NT, 36], F32, LG)
                dump("d1f", d1f[:], [128, NT], F32, ["d1f"])
                dump("d2f", d2f[:], [128, NT], F32, ["d2f"])
                dump("g1", g1[:], [128, NT], F32, ["g1"])
                dump("g2", g2[:], [128, NT], F32, ["g2"])
            for t in range(NT):
                b = t % 2
                add("sp", lambda e, t=t, b=b: e.dma_start(out=xg[b][:], in_=xn2_d[t * 128:(t + 1) * 128, :]), reads=[("xn2_d", t)], writes=[("xg", b)], dma=True)
                for dxi, nm in ((d1i, "d1i"), (d2i, "d2i")):
                    add("pool", lambda e, t=t, b=b, dxi=dxi: e.indirect_dma_start(
                        out=xdisp_d, out_offset=bass.IndirectOffsetOnAxis(ap=dxi[:, t:t + 1], axis=0), in_=xg[b][:], in_offset=None),
                        reads=[("xg", b), nm], writes=["xdisp_d"], dma=True)
            sch.flush()

        if upto >= 7:
          with ExitStack() as st:
            xs = [sb(st, "xs%d" % i, [128, 2, D], BF16) for i in range(2)]
            xT = [sb(st, "xTe%d" % i, [128, 16, CAP], BF16) for i in range(2)]
            hT = sb(st, "hT", [128, 8, CAP], BF16)
            sgl = [sb(st, "sgl%d" % i, [128, CAP], F32) for i in range(2)]
            ys = [sb(st, "ys%d" % i, [128, D], F32) for i in range(4)]
            pTe = [ps(st, "pTe%d" % i, [128, 1024], BF16) for i in range(2)]
            psG = [ps(st, "psG%d" % i, [128, 512], F32) for i in range(4)]
            psY = [ps(st, "psY%d" % i, [128, 512], F32) for i in range(2)]

            NE = int(os.environ.get("MOE_EXPERTS", NEXP))
            evc = [0]
            gcnt = [0]
            ycnt = [0]
            def xs_load(ex):
                xb = ex % 2
                add("sp", lambda e: e.dma_start(out=xs[xb][:], in_=xdisp_d[ex * CAP:(ex + 1) * CAP, :].rearrange("(s p) d -> p s d", p=128)),
                    reads=["xdisp_d"], writes=[("xs", xb)], dma=True)

            xs_load(0)
            for ex in range(NE):
                xb = ex % 2
                units = {}
                units["g0"] = get_unit(ex, "g0", w_eg[ex], False, 0)
                units["u0"] = get_unit(ex, "u0", w_eu[ex], False, 0)
                for s_ in range(2):
                    for hh in range(2):
                        pt = evc[0] % 2
                        evc[0] += 1

                        def trx(e, s_=s_, hh=hh, pt=pt, xb=xb):
                            ins = None
                            for c8 in range(8):
                                c = hh * 8 + c8
                                ins = e.transpose(out=pTe[pt][:, c8 * 128:(c8 + 1) * 128], in_=xs[xb][:, s_, c * 128:(c + 1) * 128], identity=ident)
                            return ins
                        add("pe", trx, reads=[("xs", xb), "cb"], writes=[("pTe", pt)])
                        dstx = xT[xb][:, hh * 8:(hh + 1) * 8, s_ * 128:(s_ + 1) * 128]
                        srcx = pTe[pt][:].rearrange("p (c n) -> p c n", c=8)
                        if pt == 0:
                            add("act", lambda e, dstx=dstx, srcx=srcx: e.activation(out=dstx, in_=srcx, func=AF.Copy),
                                reads=[("pTe", pt)], writes=[("xT", xb, s_, hh)])
                        else:
                            add("dve", lambda e, dstx=dstx, srcx=srcx: e.tensor_copy(out=dstx, in_=srcx),
                                reads=[("pTe", pt)], writes=[("xT", xb, s_, hh)])
                XTK = [("xT", xb, a, c) for a in range(2) for c in range(2)]
                if ex + 1 < NE:
                    xs_load(ex + 1)
                for hf in range(2):
                    if hf == 1:
                        units["g1"] = get_unit(ex, "g1", w_eg[ex], False, 1)
                        units["u1"] = get_unit(ex, "u1", w_eu[ex], False, 1)
                    ug = units["g%d" % hf]
                    uu = units["u%d" % hf]
                    for fcl in range(4):
                        fc = hf * 4 + fcl
                        pg = (gcnt[0] % 2) * 2
                        gcnt[0] += 1
                        for which, un in ((0, ug), (1, uu)):
                            def fG(e, which=which, un=un, fcl=fcl, pg=pg, xb=xb):
                                ins = None
                                wv = wu[un][:].rearrange("p (c f) -> p c f", c=16)
                                for c in range(16):
                                    ins = e.matmul(psG[pg + which][:, 0:CAP], lhsT=wv[:, c, fcl * 128:(fcl + 1) * 128], rhs=xT[xb][:, c, :],
                                                   start=(c == 0), stop=(c == 15))
                                return ins
                            add("pe", fG, reads=[("wu", un)] + XTK, writes=[("psG", pg + which)])
                        q = fc % 2
                        add("act", lambda e, pg=pg, q=q: e.activation(out=sgl[q][:], in_=psG[pg][:, 0:CAP], func=AF.Silu),
                            reads=[("psG", pg)], writes=[("sgl", q)])
                        add("dve", lambda e, pg=pg, q=q, fc=fc: e.tensor_tensor(out=hT[:, fc, :], in0=psG[pg + 1][:, 0:CAP], in1=sgl[q][:], op=ALU.mult),
                            reads=[("psG", pg + 1), ("sgl", q)], writes=[("hT", fc)])
                HTK = [("hT", fc) for fc in range(8)]
                yb = [(ycnt[0] * 2 + s_) % 4 for s_ in range(2)]
                ycnt[0] += 1
                for dh in range(2):
                    ud = get_unit(ex, "d%d" % dh, w_ed[ex], True, dh)
                    for s_ in range(2):
                        for nb in range(2):
                            py = (s_ * 2 + nb) % 2

                            def fY(e, ud=ud, s_=s_, nb=nb, py=py):
                                ins = None
                                wv = wu[ud][:].rearrange("p (c f) -> p c f", c=8)
                                for fc in range(8):
                                    ins = e.matmul(psY[py][:, :], lhsT=hT[:, fc, s_ * 128:(s_ + 1) * 128], rhs=wv[:, fc, nb * 512:(nb + 1) * 512],
                                                   start=(fc == 0), stop=(fc == 7))
                                return ins
                            add("pe", fY, reads=[("wu", ud)] + HTK, writes=[("psY", py)])
                            col = dh * 1024 + nb * 512
                            ydst = ys[yb[s_]][:, col:col + 512]
                            if nb == 0:
                                add("act", lambda e, ydst=ydst, py=py: e.activation(out=ydst, in_=psY[py][:, :], func=AF.Copy),
                                    reads=[("psY", py)], writes=[("ys", yb[s_], dh, nb)])
                            else:
                                add("dve", lambda e, ydst=ydst, py=py: e.tensor_copy(out=ydst, in_=psY[py][:, :]),
                                    reads=[("psY", py)], writes=[("ys", yb[s_], dh, nb)])
                for s_ in range(2):
                    add("sp", lambda e, ex=ex, s_=s_, ybs=yb[s_]: e.dma_start(out=ydisp_d[ex * CAP + s_ * 128:ex * CAP + (s_ + 1) * 128, :], in_=ys[ybs][:]),
                        reads=[("ys", yb[s_], dh, nb) for dh in range(2) for nb in range(2)], writes=["ydisp_d"], dma=True)
            sch.flush()

        GW.close()
        if upto >= 8:
          with ExitStack() as st:
            y1 = [sb(st, "y1_%d" % i, [128, D], F32) for i in range(3)]
            y2 = [sb(st, "y2_%d" % i, [128, D], F32) for i in range(3)]
            hh1 = [sb(st, "hh1_%d" % i, [128, D], F32) for i in range(3)]
            ot = [sb(st, "ot%d" % i, [128, D], F32) for i in range(3)]
            junk4 = sb(st, "junk4", [128, D], BF16)
            gfin = sb(st, "gfin", [128, D], F32)
            ss3 = sb(st, "ss3", [128, NT], F32)
            rs3 = sb(st, "rs3", [128, NT], F32)
            add("sp", lambda e: e.dma_start(out=gfin[:], in_=g_fin[0:1, :].to_broadcast([128, D])), writes=["gfin"], dma=True)
            def h_load(t):
                b = t % 3
                rows = slice(t * 128, (t + 1) * 128)
                add("sp", lambda e: e.dma_start(out=hh1[b][:], in_=h1_d[rows, :]), reads=[("h1_d", t)], writes=[("hh1", b)], dma=True)

            h_load(0)
            h_load(1)
            for t in range(NT):
                b = t % 3
                rows = slice(t * 128, (t + 1) * 128)
                add("pool", lambda e, t=t, b=b: e.indirect_dma_start(
                    out=y1[b][:], out_offset=None, in_=ydisp_d, in_offset=bass.IndirectOffsetOnAxis(ap=d1i[:, t:t + 1], axis=0)),
                    reads=["ydisp_d", "d1i"], writes=[("y1", b)], dma=True)
                add("pool", lambda e, t=t, b=b: e.indirect_dma_start(
                    out=y2[b][:], out_offset=None, in_=ydisp_d, in_offset=bass.IndirectOffsetOnAxis(ap=d2i[:, t:t + 1], axis=0)),
                    reads=["ydisp_d", "d2i"], writes=[("y2", b)], dma=True)
                add("dve", lambda e, t=t, b=b: e.scalar_tensor_tensor(out=hh1[b][:], in0=y1[b][:], scalar=g1[:, t:t + 1], in1=hh1[b][:],
                                                                      op0=ALU.mult, op1=ALU.add),
                    reads=[("y1", b), ("hh1", b), "g1"], writes=[("hh1", b)])
                add("dve", lambda e, t=t, b=b: e.scalar_tensor_tensor(out=hh1[b][:], in0=y2[b][:], scalar=g2[:, t:t + 1], in1=hh1[b][:],
                                                                      op0=ALU.mult, op1=ALU.add),
                    reads=[("y2", b), ("hh1", b), "g2"], writes=[("hh1", b)])
                add("act", lambda e, t=t, b=b: e.activation(out=junk4[:], in_=hh1[b][:], func=AF.Square, accum_out=ss3[:, t:t + 1]),
                    reads=[("hh1", b)], writes=["junk4", ("ss3", t)])
                add("act", lambda e, t=t: e.activation(out=rs3[:, t:t + 1], in_=ss3[:, t:t + 1], func=AF.Sqrt, scale=1.0 / D, bias=EPS),
                    reads=[("ss3", t)], writes=[("rs3", t)])
                add("dve", lambda e, t=t: e.reciprocal(out=rs3[:, t:t + 1], in_=rs3[:, t:t + 1]), reads=[("rs3", t)], writes=[("rs3", t)])
                add("dve", lambda e, t=t, b=b: e.scalar_tensor_tensor(out=ot[b][:], in0=hh1[b][:], scalar=rs3[:, t:t + 1], in1=gfin[:],
                                                                      op0=ALU.mult, op1=ALU.mult),
                    reads=[("hh1", b), ("rs3", t), "gfin"], writes=[("ot", b)])
                if t + 2 < NT:
                    h_load(t + 2)
                add("sp", lambda e, b=b, rows=rows: e.dma_start(out=out[rows, :], in_=ot[b][:]), reads=[("ot", b)], writes=[("out", t)], dma=True)
            sch.flush()
    return nc


_NC_CACHE = {}


def kernel(**inputs):
    x = np.asarray(inputs["x"], np.float32)
    B = x.shape[0]
    if "nc" not in _NC_CACHE:
        _NC_CACHE["nc"] = build()
    nc = _NC_CACHE["nc"]
    cbv, cfv, _ = make_consts()
    shared = {"cb": cbv, "cf": cfv}
    for k, v in inputs.items():
        if k == "x":
            continue
        a = np.asarray(v, np.float32)
        if k == "g_final":
            a = a.reshape(1, -1)
        else:
            a = a[0]
            if a.ndim == 1:
                a = a.reshape(1, -1)
        shared[k] = np.ascontiguousarray(a)
    in_maps = []
    for b in range(B):
        m = dict(shared)
        m["x"] = np.ascontiguousarray(x[b])
        in_maps.append(m)
    res = run_bass_kernel_spmd(nc, in_maps, core_ids=list(range(B)))
    return np.stack([np.asarray(r["out"], np.float32) for r in res.results], axis=0)
```
